# Optimizing a Trainium2 kernel written in Bass

```python
import math
import jax
import jax.numpy as jnp
from jax import lax
import numpy as np

D_MODEL = 1024
BATCH = 16
SEQ = 2048
DEPTH = 1

GRID_W = 64
CTX_LEN = 256
D_GLA = 512
D_S5 = 512
GLA_HEADS = 4
GLA_DK_HEAD = 64
GLA_DV_HEAD = D_GLA // GLA_HEADS
GLA_DK = GLA_HEADS * GLA_DK_HEAD
GLA_DV = D_GLA
GLA_GATE_RANK = 16
GLA_GATE_TAU = 16.0
GLA_CHUNK = GRID_W
S5_GROUP_CH = 16
S5_GROUPS = D_S5 // S5_GROUP_CH
S5_STATE = 64
D_IN = 2 * GLA_DK + 2 * GLA_DV + 2 * GLA_GATE_RANK + D_S5
N_EXPERTS = 256
TOP_K = 8
N_EXPERT_GROUPS = 8
TOPK_GROUPS = 4
D_EXPERT = 256
D_SHARED = 256
ROUTE_SCALE = 2.5
MOE_BLOCK = 128
EPS = 1e-6

kernel_name = 'hybrid_gla_s5_moe_prefix_block'


def rmsnorm(x, g):
    xf = x.astype(jnp.float32)
    y = xf * lax.rsqrt(jnp.mean(xf * xf, axis=-1, keepdims=True) + EPS)
    return (y * g.astype(jnp.float32)).astype(x.dtype)


def adaln(cvec, w, b):
    mod = (jax.nn.silu(cvec) @ w + b)[..., None, :]
    return jnp.split(mod, 6, axis=-1)


def modulate(h, shift, scale):
    return h * (1.0 + scale) + shift


def split_heads(t, head_dim):
    b, l, _ = t.shape
    return t.reshape(b, l, -1, head_dim).transpose(0, 2, 1, 3)


def gla_log_gate(lr, w, b):
    return jax.nn.log_sigmoid((lr @ w + b).astype(jnp.float32)) / GLA_GATE_TAU


def gla_chunked(q, k, v, log_a, s0):
    bsz, nh, seq, dk = q.shape
    dv = v.shape[-1]
    n_chunks = seq // GLA_CHUNK
    cshape = (bsz, nh, n_chunks, GLA_CHUNK)
    cum = jnp.cumsum(log_a.reshape(cshape + (dk,)), axis=3)
    cum_last = cum[:, :, :, -1:, :]
    qc = q.reshape(cshape + (dk,)) * jnp.exp(cum)
    kc = k.reshape(cshape + (dk,))
    k_intra = kc * jnp.exp(-cum)
    k_state = kc * jnp.exp(cum_last - cum)
    vc = v.reshape(cshape + (dv,))
    tri = jnp.tril(jnp.ones((GLA_CHUNK, GLA_CHUNK), dtype=bool))
    scores = jnp.where(tri, jnp.einsum('bhncd,bhnsd->bhncs', qc, k_intra), 0.0)
    o_intra = jnp.einsum('bhncs,bhnsv->bhncv', scores, vc)
    decay = jnp.exp(cum_last[:, :, :, 0, :])

    def step(state, inp):
        q_n, k_n, v_n, d_n = inp
        o_n = jnp.einsum('bhcd,bhdv->bhcv', q_n, state)
        state = d_n[..., None] * state + jnp.einsum('bhcd,bhcv->bhdv', k_n, v_n)
        return state, o_n

    xs = tuple(jnp.moveaxis(t, 2, 0) for t in (qc, k_state, vc, decay))
    s_fin, o_inter = lax.scan(step, s0, xs)
    o = o_intra + jnp.moveaxis(o_inter, 0, 2)
    return o.reshape(bsz, nh, seq, dv), s_fin


def s5_discretize(lam_re, lam_im, log_step, b_re, b_im):
    lam_re = lam_re.astype(jnp.float32)
    lam_im = lam_im.astype(jnp.float32)
    step = jnp.exp(log_step.astype(jnp.float32))[:, None]
    mag = jnp.exp(lam_re * step)
    a_re = mag * jnp.cos(lam_im * step)
    a_im = mag * jnp.sin(lam_im * step)
    den = lam_re * lam_re + lam_im * lam_im
    f_re = ((a_re - 1.0) * lam_re + a_im * lam_im) / den
    f_im = (a_im * lam_re - (a_re - 1.0) * lam_im) / den
    b_re = b_re.astype(jnp.float32)
    b_im = b_im.astype(jnp.float32)
    bb_re = f_re[..., None] * b_re - f_im[..., None] * b_im
    bb_im = f_re[..., None] * b_im + f_im[..., None] * b_re
    return (a_re, a_im), (bb_re, bb_im)


def s5_scan(abar, bbar, u_g, x0, reverse):
    a_re, a_im = abar
    u32 = u_g.astype(jnp.float32)
    bu_re = jnp.einsum('blgh,gph->blgp', u32, bbar[0])
    bu_im = jnp.einsum('blgh,gph->blgp', u32, bbar[1])
    if x0 is not None:
        x0_re, x0_im = x0
        pos = -1 if reverse else 0
        bu_re = bu_re.at[:, pos].add(a_re * x0_re - a_im * x0_im)
        bu_im = bu_im.at[:, pos].add(a_re * x0_im + a_im * x0_re)
    seq = bu_re.shape[1]
    ar = jnp.broadcast_to(a_re, (1, seq) + a_re.shape)
    ai = jnp.broadcast_to(a_im, (1, seq) + a_im.shape)

    def combine(earlier, later):
        ar1, ai1, br1, bi1 = earlier
        ar2, ai2, br2, bi2 = later
        return (ar2 * ar1 - ai2 * ai1,
                ar2 * ai1 + ai2 * ar1,
                ar2 * br1 - ai2 * bi1 + br2,
                ar2 * bi1 + ai2 * br1 + bi2)

    _, _, x_re, x_im = lax.associative_scan(combine, (ar, ai, bu_re, bu_im), reverse=reverse, axis=1)
    return x_re, x_im


def s5_readout(xs, c_re, c_im):
    x_re, x_im = xs
    return (jnp.einsum('blgp,ghp->blgh', x_re, c_re.astype(jnp.float32))
            - jnp.einsum('blgp,ghp->blgh', x_im, c_im.astype(jnp.float32)))


def stream_mix(proj, lp, init_states, with_output):
    bsz = proj.shape[0]
    o1 = GLA_DK
    o2 = o1 + GLA_DK
    o3 = o2 + GLA_DV
    o4 = o3 + GLA_DV
    o5 = o4 + GLA_GATE_RANK
    o6 = o5 + GLA_GATE_RANK
    q, k, v, g_out, lr_f, lr_b, u = jnp.split(proj, [o1, o2, o3, o4, o5, o6], axis=-1)
    q = split_heads(q, GLA_DK_HEAD) * (GLA_DK_HEAD ** -0.5)
    k = split_heads(k, GLA_DK_HEAD)
    v = split_heads(v, GLA_DV_HEAD)
    la_f = split_heads(gla_log_gate(lr_f, lp['gla_wa_f'], lp['gla_ba_f']), GLA_DK_HEAD)
    la_b = split_heads(gla_log_gate(lr_b, lp['gla_wa_b'], lp['gla_ba_b']), GLA_DK_HEAD)
    if init_states is None:
        zero = jnp.zeros((bsz, GLA_HEADS, GLA_DK_HEAD, GLA_DV_HEAD), jnp.float32)
        gla_s0_f, gla_s0_b, s5_x0_f, s5_x0_b = zero, zero, None, None
    else:
        gla_s0_f, gla_s0_b, s5_x0_f, s5_x0_b = init_states
    o_f, gla_sf = gla_chunked(q, k, v, la_f, gla_s0_f)
    o_b, gla_sb = gla_chunked(jnp.flip(q, 2), jnp.flip(k, 2), jnp.flip(v, 2), jnp.flip(la_b, 2), gla_s0_b)
    u_g = u.reshape(bsz, -1, S5_GROUPS, S5_GROUP_CH)
    abar_f, bbar_f = s5_discretize(lp['s5_lam_re_f'], lp['s5_lam_im_f'], lp['s5_log_step_f'], lp['s5_b_re'], lp['s5_b_im'])
    abar_b, bbar_b = s5_discretize(lp['s5_lam_re_b'], lp['s5_lam_im_b'], lp['s5_log_step_b'], lp['s5_b_re'], lp['s5_b_im'])
    x_f = s5_scan(abar_f, bbar_f, u_g, s5_x0_f, False)
    x_b = s5_scan(abar_b, bbar_b, u_g, s5_x0_b, True)
    states = (gla_sf, gla_sb, (x_f[0][:, -1], x_f[1][:, -1]), (x_b[0][:, 0], x_b[1][:, 0]))
    if not with_output:
        return None, states
    o = rmsnorm(o_f + jnp.flip(o_b, 2), lp['gla_norm_g'])
    o = o.transpose(0, 2, 1, 3).reshape(bsz, -1, GLA_DV)
    gla_out = o * jax.nn.silu(g_out.astype(jnp.float32))
    y = s5_readout(x_f, lp['s5_c_re_f'], lp['s5_c_im_f']) + s5_readout(x_b, lp['s5_c_re_b'], lp['s5_c_im_b'])
    y = y.reshape(bsz, -1, D_S5) + lp['s5_d'] * u
    z = jax.nn.gelu(y)
    s5_out = z * jax.nn.sigmoid(z @ lp['s5_glu_w'] + lp['s5_glu_b'])
    return jnp.concatenate([gla_out, s5_out], axis=-1).astype(proj.dtype), states


def moe_ffn(h, router_w, router_b, w_gate, w_up, w_down, sh_gate, sh_up, sh_down):
    shp = h.shape
    t = h.reshape(-1, shp[-1])
    n_tok = t.shape[0]
    scores = jax.nn.sigmoid((t @ router_w).astype(jnp.float32))
    biased = scores + router_b.astype(jnp.float32)
    grp = biased.reshape(n_tok, N_EXPERT_GROUPS, N_EXPERTS // N_EXPERT_GROUPS)
    grp_score = lax.top_k(grp, 2)[0].sum(-1)
    _, top_grp = lax.top_k(grp_score, TOPK_GROUPS)
    grp_mask = jax.nn.one_hot(top_grp, N_EXPERT_GROUPS, dtype=jnp.float32).sum(1) > 0
    exp_mask = jnp.repeat(grp_mask, N_EXPERTS // N_EXPERT_GROUPS, axis=1)
    _, top_e = lax.top_k(jnp.where(exp_mask, biased, -jnp.inf), TOP_K)
    wts = jnp.take_along_axis(scores, top_e, axis=1)
    wts = wts / jnp.sum(wts, axis=-1, keepdims=True) * ROUTE_SCALE
    n_assign = n_tok * TOP_K
    flat_e = top_e.reshape(-1)
    order = jnp.argsort(flat_e)
    sorted_e = flat_e[order]
    counts = jnp.bincount(flat_e, length=N_EXPERTS)
    padded = (counts + MOE_BLOCK - 1) // MOE_BLOCK * MOE_BLOCK
    padded_end = jnp.cumsum(padded)
    padded_start = padded_end - padded
    start = jnp.cumsum(counts) - counts
    dest = padded_start[sorted_e] + jnp.arange(n_assign, dtype=jnp.int32) - start[sorted_e]
    n_rows = -(-(n_assign + N_EXPERTS * (MOE_BLOCK - 1)) // MOE_BLOCK) * MOE_BLOCK
    n_blocks = n_rows // MOE_BLOCK
    row_tok = jnp.zeros((n_rows,), jnp.int32).at[dest].set((order // TOP_K).astype(jnp.int32))
    row_w = jnp.zeros((n_rows,), jnp.float32).at[dest].set(wts.reshape(-1)[order])
    blk_e = jnp.minimum(jnp.searchsorted(padded_end, jnp.arange(n_blocks, dtype=jnp.int32) * MOE_BLOCK, side='right'), N_EXPERTS - 1)

    def body(acc, blk):
        tok, w, e = blk
        xb = t[tok]
        hid = jax.nn.silu(xb @ w_gate[e]) * (xb @ w_up[e])
        yb = (hid @ w_down[e]) * w[:, None]
        return acc.at[tok].add(yb.astype(acc.dtype)), None

    routed, _ = lax.scan(body, jnp.zeros_like(t),
                         (row_tok.reshape(n_blocks, MOE_BLOCK), row_w.reshape(n_blocks, MOE_BLOCK), blk_e))
    shared = (jax.nn.silu(t @ sh_gate) * (t @ sh_up)) @ sh_down
    return (routed + shared).reshape(shp)


def setup_inputs(seed: int = 0) -> dict:
    key = jax.random.key(seed)
    ks = iter(jax.random.split(key, 64))

    def nrm(shape, scale):
        return jax.random.normal(next(ks), shape, jnp.float32) * scale

    def gain(shape):
        return 1.0 + 0.02 * jax.random.normal(next(ks), shape, jnp.float32)

    lam_im_base = jnp.pi * jnp.arange(S5_STATE, dtype=jnp.float32)
    log_lo, log_hi = math.log(1e-3), math.log(1e-1)
    return {
        'x': nrm((BATCH, SEQ, D_MODEL), 1.0),
        'c': nrm((BATCH, D_MODEL), 1.0),
        'ctx': nrm((BATCH, CTX_LEN, D_MODEL), 1.0),
        'c_ctx': nrm((D_MODEL,), 1.0),
        'ada_w': nrm((DEPTH, D_MODEL, 6 * D_MODEL), 0.5 * D_MODEL ** -0.5),
        'ada_b': nrm((DEPTH, 6 * D_MODEL), 0.02),
        'norm1_g': gain((DEPTH, D_MODEL)),
        'norm2_g': gain((DEPTH, D_MODEL)),
        'w_in': nrm((DEPTH, D_MODEL, D_IN), D_MODEL ** -0.5),
        'gla_wa_f': nrm((DEPTH, GLA_GATE_RANK, GLA_DK), GLA_GATE_RANK ** -0.5),
        'gla_ba_f': nrm((DEPTH, GLA_DK), 0.1),
        'gla_wa_b': nrm((DEPTH, GLA_GATE_RANK, GLA_DK), GLA_GATE_RANK ** -0.5),
        'gla_ba_b': nrm((DEPTH, GLA_DK), 0.1),
        'gla_norm_g': gain((DEPTH, GLA_DV_HEAD)),
        's5_lam_re_f': -0.5 + nrm((DEPTH, S5_GROUPS, S5_STATE), 0.01),
        's5_lam_im_f': lam_im_base + nrm((DEPTH, S5_GROUPS, S5_STATE), 0.01),
        's5_log_step_f': jax.random.uniform(next(ks), (DEPTH, S5_GROUPS), jnp.float32, log_lo, log_hi),
        's5_lam_re_b': -0.5 + nrm((DEPTH, S5_GROUPS, S5_STATE), 0.01),
        's5_lam_im_b': lam_im_base + nrm((DEPTH, S5_GROUPS, S5_STATE), 0.01),
        's5_log_step_b': jax.random.uniform(next(ks), (DEPTH, S5_GROUPS), jnp.float32, log_lo, log_hi),
        's5_b_re': nrm((DEPTH, S5_GROUPS, S5_STATE, S5_GROUP_CH), (2.0 * S5_GROUP_CH) ** -0.5),
        's5_b_im': nrm((DEPTH, S5_GROUPS, S5_STATE, S5_GROUP_CH), (2.0 * S5_GROUP_CH) ** -0.5),
        's5_c_re_f': nrm((DEPTH, S5_GROUPS, S5_GROUP_CH, S5_STATE), (2.0 * S5_STATE) ** -0.5),
        's5_c_im_f': nrm((DEPTH, S5_GROUPS, S5_GROUP_CH, S5_STATE), (2.0 * S5_STATE) ** -0.5),
        's5_c_re_b': nrm((DEPTH, S5_GROUPS, S5_GROUP_CH, S5_STATE), (2.0 * S5_STATE) ** -0.5),
        's5_c_im_b': nrm((DEPTH, S5_GROUPS, S5_GROUP_CH, S5_STATE), (2.0 * S5_STATE) ** -0.5),
        's5_d': nrm((DEPTH, D_S5), 1.0),
        's5_glu_w': nrm((DEPTH, D_S5, D_S5), D_S5 ** -0.5),
        's5_glu_b': nrm((DEPTH, D_S5), 0.02),
        'w_out': nrm((DEPTH, D_MODEL, D_MODEL), D_MODEL ** -0.5),
        'router_w': nrm((DEPTH, D_MODEL, N_EXPERTS), D_MODEL ** -0.5),
        'router_b': nrm((DEPTH, N_EXPERTS), 0.01),
        'exp_w_gate': nrm((DEPTH, N_EXPERTS, D_MODEL, D_EXPERT), D_MODEL ** -0.5),
        'exp_w_up': nrm((DEPTH, N_EXPERTS, D_MODEL, D_EXPERT), D_MODEL ** -0.5),
        'exp_w_down': nrm((DEPTH, N_EXPERTS, D_EXPERT, D_MODEL), D_EXPERT ** -0.5),
        'sh_w_gate': nrm((DEPTH, D_MODEL, D_SHARED), D_MODEL ** -0.5),
        'sh_w_up': nrm((DEPTH, D_MODEL, D_SHARED), D_MODEL ** -0.5),
        'sh_w_down': nrm((DEPTH, D_SHARED, D_MODEL), D_SHARED ** -0.5),
        'final_norm_g': gain((D_MODEL,)),
    }


def reference(x, c, ctx, c_ctx, ada_w, ada_b, norm1_g, norm2_g, w_in,
              gla_wa_f, gla_ba_f, gla_wa_b, gla_ba_b, gla_norm_g,
              s5_lam_re_f, s5_lam_im_f, s5_log_step_f, s5_lam_re_b, s5_lam_im_b, s5_log_step_b,
              s5_b_re, s5_b_im, s5_c_re_f, s5_c_im_f, s5_c_re_b, s5_c_im_b, s5_d, s5_glu_w, s5_glu_b,
              w_out, router_w, router_b, exp_w_gate, exp_w_up, exp_w_down,
              sh_w_gate, sh_w_up, sh_w_down, final_norm_g):
    h_ctx = ctx
    for i in range(DEPTH):
        lp = {
            'gla_wa_f': gla_wa_f[i], 'gla_ba_f': gla_ba_f[i],
            'gla_wa_b': gla_wa_b[i], 'gla_ba_b': gla_ba_b[i], 'gla_norm_g': gla_norm_g[i],
            's5_lam_re_f': s5_lam_re_f[i], 's5_lam_im_f': s5_lam_im_f[i], 's5_log_step_f': s5_log_step_f[i],
            's5_lam_re_b': s5_lam_re_b[i], 's5_lam_im_b': s5_lam_im_b[i], 's5_log_step_b': s5_log_step_b[i],
            's5_b_re': s5_b_re[i], 's5_b_im': s5_b_im[i],
            's5_c_re_f': s5_c_re_f[i], 's5_c_im_f': s5_c_im_f[i],
            's5_c_re_b': s5_c_re_b[i], 's5_c_im_b': s5_c_im_b[i],
            's5_d': s5_d[i], 's5_glu_w': s5_glu_w[i], 's5_glu_b': s5_glu_b[i],
        }
        last = i + 1 == DEPTH
        sh1, sc1, g1, sh2, sc2, g2 = adaln(c, ada_w[i], ada_b[i])
        csh1, csc1, cg1, csh2, csc2, cg2 = adaln(c_ctx, ada_w[i], ada_b[i])
        hc = modulate(rmsnorm(h_ctx, norm1_g[i]), csh1, csc1)
        mix_c, ctx_states = stream_mix(hc @ w_in[i], lp, None, not last)
        hx = modulate(rmsnorm(x, norm1_g[i]), sh1, sc1)
        mix_x, _ = stream_mix(hx @ w_in[i], lp, ctx_states, True)
        x = x + g1 * (mix_x @ w_out[i])
        hx2 = modulate(rmsnorm(x, norm2_g[i]), sh2, sc2)
        x = x + g2 * moe_ffn(hx2, router_w[i], router_b[i], exp_w_gate[i], exp_w_up[i], exp_w_down[i],
                             sh_w_gate[i], sh_w_up[i], sh_w_down[i])
        if not last:
            h_ctx = h_ctx + cg1 * (mix_c @ w_out[i])
            hc2 = modulate(rmsnorm(h_ctx, norm2_g[i]), csh2, csc2)
            h_ctx = h_ctx + cg2 * moe_ffn(hc2, router_w[i], router_b[i], exp_w_gate[i], exp_w_up[i], exp_w_down[i],
                                          sh_w_gate[i], sh_w_up[i], sh_w_down[i])
    return rmsnorm(x, final_norm_g)
```

```python
import numpy as np
import math
from contextlib import ExitStack
import concourse.bass as bass
import concourse.mybir as mybir
from concourse.bass_utils import run_bass_kernel_spmd

F32 = mybir.dt.float32
BF16 = mybir.dt.bfloat16
I32 = mybir.dt.int32
U32 = mybir.dt.uint32
ALU = mybir.AluOpType
AF = mybir.ActivationFunctionType
AX = mybir.AxisListType

D = 1024
DIN = 2080
NE = 256
EPS = 1e-6
NDSEM = 24
import os
FASTPE = tuple(os.environ.get('FASTPE', 'mm,tr').split(','))


class Prog:
    ENG = ['pe', 'dve', 'act', 'pool', 'sp']

    def __init__(self, nc):
        self.nc = nc
        self.ops = {e: [] for e in self.ENG}
        self.res = {}
        self.dmas = {'dma': [], 'sw': []}
        self.pending = {e: [] for e in self.ENG}
        self.last = {e: None for e in self.ENG}

    def _key(self, item):
        if isinstance(item, tuple):
            return (item[0].tensor.name, item[1])
        return (item.tensor.name, None)

    pemode = None

    def op(self, eng, fn, reads=(), writes=(), dma=False, pemode=None):
        deps = list(self.pending[eng])
        self.pending[eng] = []
        rk = [self._key(i) for i in reads]
        wk = [self._key(i) for i in writes]
        for k in rk:
            r = self.res.get(k)
            if r and r[0] is not None:
                deps.append(r[0])
        for k in wk:
            r = self.res.get(k)
            if r:
                if r[0] is not None:
                    deps.append(r[0])
                for en, ix in r[1].items():
                    deps.append((en, ix))
                deps.extend(r[2])
        o = {'fn': fn, 'dma': None, 'signal': False, 'eng': eng}
        if dma:
            kind = 'sw' if eng == 'pool' else 'dma'
            lst = self.dmas[kind]
            n = len(lst)
            tok = (kind, n)
            if n >= NDSEM:
                deps.append((kind, n - NDSEM))
            lst.append(o)
            o['dma'] = kind
            o['dn'] = n
        else:
            tok = (eng, len(self.ops[eng]))
        o['deps'] = set(deps)
        o['deps'].discard(tok)
        if eng == 'pe' and not dma:
            def slow_(m):
                return m is None or 'float32' in m[3] or m[1] < 128 or m[2] < 128 or (m[0] not in FASTPE)
            slow = slow_(pemode) or slow_(self.pemode)
            if not slow:
                o['deps'] = {d for d in o['deps'] if d[0] != 'pe'}
            if (slow or pemode != self.pemode) and self.last['pe'] is not None:
                o['deps'].add(self.last['pe'])
                if not slow:
                    o['drain'] = True
            self.pemode = pemode
        self.ops[eng].append(o)
        if not dma:
            self.last[eng] = tok
        for k in rk:
            r = self.res.setdefault(k, [None, {}, []])
            if dma:
                r[2].append(tok)
            else:
                r[1][eng] = tok[1]
        for k in wk:
            self.res[k] = [tok, {}, []]
        return tok

    def barrier(self):
        toks = [t for t in self.last.values() if t is not None]
        for kind, lst in self.dmas.items():
            toks += [(kind, i) for i in range(max(0, len(lst) - NDSEM), len(lst))]
        for e in self.ENG:
            self.pending[e] = list(toks)
        self.res = {}

    def emit(self, esems, dsems):
        nc = self.nc
        for e in self.ENG:
            for o in self.ops[e]:
                for d in o['deps']:
                    if d[0] not in ('dma', 'sw'):
                        self.ops[d[0]][d[1]]['signal'] = True
        val = {}
        for e in self.ENG:
            cnt = 0
            for i, o in enumerate(self.ops[e]):
                if o['dma']:
                    continue
                if o['signal']:
                    cnt += 1
                    val[(e, i)] = cnt
        engobj = {'pe': nc.tensor, 'dve': nc.vector, 'act': nc.scalar, 'pool': nc.gpsimd, 'sp': nc.sync}

        def tokval(d):
            if d[0] in ('dma', 'sw'):
                n = d[1]
                return (d[0], n % NDSEM), dsems[d[0]][n % NDSEM], 16 * (n // NDSEM + 1)
            return ('e', d[0]), esems[d[0]], val[d]

        def run(e):
            eo = engobj[e]
            seen = {}
            for i, o in enumerate(self.ops[e]):
                for d in sorted(o['deps'], key=str):
                    k, sem, v = tokval(d)
                    if seen.get(k, 0) >= v:
                        continue
                    seen[k] = v
                    eo.wait_ge(sem, v)
                if o.get('drain'):
                    eo.drain()
                ins = o['fn'](eo)
                if o['dma']:
                    n = o['dn']
                    ins.then_inc(dsems[o['dma']][n % NDSEM], 16)
                elif o['signal']:
                    ins.then_inc(esems[e], 1)
        return run

    def final_wait_all(self):
        self.barrier()
        self.op('sp', lambda e: e.nop(), [], [])


def build_program(T=2048, CT=256, NB=2, debug=None, upto=99):
    nc = bass.Bass("TRN2", target_bir_lowering=False)
    P = Prog(nc)
    NT = T // 128
    NCT = CT // 128
    NKX = T // 8
    NKC = CT // 8
    NK = NKX + NKC
    NCTL = max(1, T // 1024)
    CPT = min(128, NKX)
    NSET = NCTL * 8
    CAP = 128 * NB
    NROW = 128 * 2 * NE

    def din(name, shape, dt=F32):
        return nc.dram_tensor(name, list(shape), dt, kind="ExternalInput").ap()

    def dscr(name, shape, dt=F32):
        return nc.dram_tensor(name, list(shape), dt, kind="Internal").ap()

    x = din("x", [2, T, D]); cvec = din("c", [2, D]); ctx = din("ctx", [2, CT, D]); c_ctx = din("c_ctx", [1, D])
    ada_w = din("ada_w", [D, 6 * D]); ada_b = din("ada_b", [1, 6 * D])
    norm1_g = din("norm1_g", [1, D]); norm2_g = din("norm2_g", [1, D])
    w_in = din("w_in", [D, DIN])
    gla_wa = [din("gla_wa_f", [16, 256]), din("gla_wa_b", [16, 256])]
    gla_ba = [din("gla_ba_f", [1, 256]), din("gla_ba_b", [1, 256])]
    gla_norm_g = din("gla_norm_g", [1, 128])
    lam_re = [din("s5_lam_re_f", [32, 64]), din("s5_lam_re_b", [32, 64])]
    lam_im = [din("s5_lam_im_f", [32, 64]), din("s5_lam_im_b", [32, 64])]
    log_step = [din("s5_log_step_f", [1, 32]), din("s5_log_step_b", [1, 32])]
    s5_b = [din("s5_b_re", [32, 64, 16]), din("s5_b_im", [32, 64, 16])]
    s5_c = [[din("s5_c_re_f", [512, 64]), din("s5_c_im_f", [512, 64])],
            [din("s5_c_re_b", [512, 64]), din("s5_c_im_b", [512, 64])]]
    s5_d = din("s5_d", [1, 512]); s5_glu_w = din("s5_glu_w", [512, 512]); s5_glu_b = din("s5_glu_b", [1, 512])
    w_out = din("w_out", [D, D]); router_w = din("router_w", [D, NE]); router_b = din("router_b", [1, NE])
    exp_wg = din("exp_w_gate", [NE * 128, 2048]); exp_wu = din("exp_w_up", [NE * 128, 2048]); exp_wd = din("exp_w_down", [NE * 128, 2048])
    sh_wg = din("sh_w_gate", [D, 256]); sh_wu = din("sh_w_up", [D, 256]); sh_wd = din("sh_w_down", [256, D])
    final_g = din("final_norm_g", [1, D])
    out = nc.dram_tensor("out", [2, T, D], F32, kind="ExternalOutput").ap()

    WIN_BF = dscr("win_bf", [D, DIN], BF16); WOUT_BF = dscr("wout_bf", [D, D], BF16)
    GLU_BF = dscr("glu_bf", [512, 512], BF16); RW_BF = dscr("rw_bf", [D, NE], BF16)
    SHG_BF = dscr("shg_bf", [D, 256], BF16); SHU_BF = dscr("shu_bf", [D, 256], BF16); SHD_BF = dscr("shd_bf", [256, D], BF16)
    MODBC = dscr("modbc", [4, 2, 128, D])
    TOEP_S = dscr("toep_s", [128, 32 * 128], BF16)
    WS_S = dscr("ws_s", [128, 32 * 2 * 2 * 64], BF16)
    WO_S = dscr("wo_s", [128, 32 * 2 * 128], BF16)
    A8_S = dscr("a8_s", [128, 128])
    U_S = dscr("u_s", [2, T, 512], BF16); UC_S = dscr("uc_s", [2, CT, 512], BF16)
    X1_S = dscr("x1_s", [2, T, D]); HX2_S = dscr("hx2_s", [2 * T, D], BF16)
    RINFO = dscr("rinfo", [NROW + 1, 2], I32)
    Y_S = dscr("y_s", [NROW + 1, D], BF16)

    dbg_outs = {}

    def dump(name, ap, shape, dt=F32):
        if debug is None or name not in debug:
            return
        t = nc.dram_tensor("dbg_" + name, list(shape), dt, kind="ExternalOutput").ap()
        dbg_outs[name] = t
        P.op('sp', lambda e, o=t, i=ap: e.dma_start(out=o, in_=i), [ap], [t], dma=True)

    es = ExitStack()

    uid = [0]

    def sb(st, name, shape, dt=F32):
        uid[0] += 1
        return st.enter_context(nc.sbuf_tensor(f"{name}_{uid[0]}", list(shape), dt))

    def ps(st, name, shape, dt=F32):
        uid[0] += 1
        return st.enter_context(nc.psum_tensor(f"{name}_{uid[0]}", list(shape), dt))

    def dma(o, i, q='sp', **kw):
        return P.op(q, lambda e, o=o, i=i, kw=kw: e.dma_start(out=o, in_=i, **kw), [i], [o], dma=True)

    def rnd_(n):
        return 32 if n <= 32 else (64 if n <= 64 else 128)

    def mm(o, l, r, st=True, sp=True, extra_r=()):
        mode = ('mm', rnd_(l.shape[0]), rnd_(int(np.prod(l.shape[1:]))), str(l.dtype))
        return P.op('pe', lambda e, o=o, l=l, r=r, st=st, sp=sp: e.matmul(o, l, r, start=st, stop=sp),
                    [l, r] + list(extra_r), [o], pemode=mode)

    def tr(o, i, ident):
        mode = ('tr', rnd_(i.shape[0]), rnd_(int(np.prod(i.shape[1:]))), str(i.dtype))
        return P.op('pe', lambda e, o=o, i=i, idn=ident: e.transpose(o, i, idn), [i, ident], [o], pemode=mode)

    def U_(a):
        return a[0] if isinstance(a, tuple) else a

    def tt(eng, o, a, b, op):
        return P.op(eng, lambda e, o=U_(o), a=U_(a), b=U_(b), op=op: e.tensor_tensor(out=o, in0=a, in1=b, op=op), [a, b], [o])

    def ts(eng, o, a, s1, s2, op0, op1=None, accum=None):
        rd = [a] + [s for s in (s1, s2) if isinstance(s, bass.AP)]
        wr = [o] + ([accum] if accum is not None else [])

        def f(e, o=o, a=a, s1=s1, s2=s2, op0=op0, op1=op1, accum=accum):
            kw = {}
            if accum is not None:
                kw['accum_out'] = accum
            if op1 is None:
                return e.tensor_scalar(out=o, in0=a, scalar1=s1, scalar2=None, op0=op0, **kw)
            return e.tensor_scalar(out=o, in0=a, scalar1=s1, scalar2=s2, op0=op0, op1=op1, **kw)
        return P.op(eng, f, rd, wr)

    def stt(o, a, s, b, op0, op1, accum=None):
        rd = [a, b] + ([s] if isinstance(s, bass.AP) else [])
        wr = [o] + ([accum] if accum is not None else [])

        def f(e, o=o, a=a, s=s, b=b, op0=op0, op1=op1, accum=accum):
            kw = {}
            if accum is not None:
                kw['accum_out'] = accum
            return e.scalar_tensor_tensor(out=o, in0=a, scalar=s, in1=b, op0=op0, op1=op1, **kw)
        return P.op('dve', f, rd, wr)

    def act(o, i, func, bias=None, scale=None, accum=None):
        rd = [i] + [s for s in (bias, scale) if isinstance(s, bass.AP)]
        wr = [o] + ([accum] if accum is not None else [])

        def f(e, o=o, i=i, func=func, bias=bias, scale=scale, accum=accum):
            kw = {}
            if bias is not None:
                kw['bias'] = bias
            if scale is not None:
                kw['scale'] = scale
            if accum is not None:
                kw['accum_out'] = accum
            return e.activation(out=o, in_=i, func=func, **kw)
        return P.op('act', f, rd, wr)

    def cp(eng, o, i):
        if eng == 'act':
            return P.op('act', lambda e, o=U_(o), i=U_(i): e.copy(out=o, in_=i), [i], [o])
        return P.op(eng, lambda e, o=U_(o), i=U_(i): e.tensor_copy(out=o, in_=i), [i], [o])

    def memset(eng, o, v):
        return P.op(eng, lambda e, o=o, v=v: e.memset(o, v), [], [o])

    def recip(o, i):
        return P.op('dve', lambda e, o=o, i=i: e.reciprocal(out=o, in_=i), [i], [o])

    def iota(o, pattern, base, cm):
        return P.op('pool', lambda e, o=o: e.iota(o, pattern=pattern, base=base, channel_multiplier=cm), [], [o])

    def rsqrt_col(o, i, scale, tmp):
        ts('dve', tmp, i, scale, EPS, ALU.mult, ALU.add)
        act(tmp, tmp, AF.Sqrt)
        recip(o, tmp)

    st0 = es
    ident_f = sb(st0, "ident_f", [128, 128]); ident_b = sb(st0, "ident_b", [128, 128], BF16)
    maskF = sb(st0, "maskF", [128, 128]); maskB = sb(st0, "maskB", [128, 128])
    SLm = sb(st0, "SLm", [128, 128]); SUm = sb(st0, "SUm", [128, 128])
    SU_b = sb(st0, "SU_b", [128, 128], BF16); ones_b = sb(st0, "ones_b", [128, 128], BF16)
    ones_f = sb(st0, "ones_f", [128, 128])
    iot = sb(st0, "iot", [128, 128], I32); iotf = sb(st0, "iotf", [128, 128])
    AB = sb(st0, "AB", [128, 4, 8, 3])
    iota(iot[:], [[1, 128]], 0, -1)
    cp('dve', iotf[:], iot[:])
    ts('dve', ident_f[:], iotf[:], 0.0, None, ALU.is_equal)
    cp('dve', ident_b[:], ident_f[:])
    ts('dve', maskF[:], iotf[:], 0.0, None, ALU.is_ge)
    ts('dve', maskB[:], iotf[:], 0.0, None, ALU.is_le)
    ts('dve', SLm[:], iotf[:], 0.0, None, ALU.is_lt)
    ts('dve', SUm[:], iotf[:], 0.0, None, ALU.is_gt)
    cp('dve', SU_b[:], SUm[:])
    memset('dve', ones_f[:], 1.0)
    memset('dve', ones_b[:], 1.0)

    with ExitStack() as st:
        stg = [sb(st, f"wstg{i}", [128, DIN]) for i in range(2)]
        stb = [sb(st, f"wstb{i}", [128, DIN], BF16) for i in range(2)]
        cnt = 0
        for (src, dst, K, N) in [(w_in, WIN_BF, D, DIN), (w_out, WOUT_BF, D, D), (s5_glu_w, GLU_BF, 512, 512),
                                 (router_w, RW_BF, D, NE), (sh_wg, SHG_BF, D, 256), (sh_wu, SHU_BF, D, 256),
                                 (sh_wd, SHD_BF, 256, D)]:
            for kc in range(K // 128):
                a = stg[cnt % 2]; b = stb[cnt % 2]
                dma(a[:, 0:N], src[kc * 128:(kc + 1) * 128, :])
                cp(['dve', 'act', 'pool'][cnt % 3], b[:, 0:N], a[:, 0:N])
                dma(dst[kc * 128:(kc + 1) * 128, :], b[:, 0:N])
                cnt += 1
    P.barrier()

    with ExitStack() as st:
        cT = sb(st, "cT", [128, 8, 3]); sT = sb(st, "sT", [128, 8, 3])
        sBC = sb(st, "sBC", [128, 2, 8, 128])
        abT = sb(st, "abT", [128, 48]); n1g = sb(st, "n1g", [128, 8]); n2g = sb(st, "n2g", [128, 8])
        modsb = sb(st, "modsb", [128, 48, 3])
        aw = [sb(st, f"aw{i}", [128, 8, 512]) for i in range(2)]
        abrow = [sb(st, f"abrow{i}", [1, 512]) for i in range(2)]
        bct = [sb(st, f"bct{i}", [128, 512]) for i in range(2)]
        n2bc = sb(st, "n2bc", [128, D])
        modps = ps(st, "modps", [128, 192])
        bcps = [ps(st, f"bcps{i}", [128, 512]) for i in range(2)]
        NCK = dict(allow_slow_non_contiguous=True)
        cT2 = sb(st, "cT2", [128, 3, 8])
        for j in range(2):
            dma(cT2[:, j, :], cvec[j:j + 1, :].rearrange("o (kc p) -> p (o kc)", p=128), **NCK)
        dma(cT2[:, 2, :], c_ctx.rearrange("o (kc p) -> p (o kc)", p=128), **NCK)
        cp('dve', cT[:], cT2[:].rearrange("p j k -> p k j"))
        dma(abT[:], ada_b.rearrange("o (fc p) -> p (o fc)", p=128), **NCK)
        dma(n1g[:], norm1_g.rearrange("o (kc p) -> p (o kc)", p=128), **NCK)
        dma(n2g[:], norm2_g.rearrange("o (kc p) -> p (o kc)", p=128), **NCK)
        dma(n2bc[:], norm2_g.partition_broadcast(128).rearrange("p o d -> p (o d)"))
        act(sT[:], cT[:], AF.Silu)
        for j in range(2):
            for kc in range(8):
                cp('dve', sBC[:, j, kc, :], sT[:, kc, j:j + 1].broadcast_to([128, 128]))
        bci = 0
        for nb in range(12):
            a = aw[nb % 2]
            dma(a[:], ada_w[:, nb * 512:(nb + 1) * 512].rearrange("(kc p) n -> p kc n", p=128))
            mi = nb // 2; half = nb % 2
            for f4 in range(4):
                fc = nb * 4 + f4
                for kc in range(8):
                    mm(modps[:, fc * 4:fc * 4 + 3], a[:, kc, f4 * 128:(f4 + 1) * 128], sT[:, kc, :], kc == 0, kc == 7)
            if mi in (2, 3, 4, 5):
                ar = abrow[nb % 2]
                dma(ar[:], ada_b[:, nb * 512:(nb + 1) * 512])
                for j in range(2):
                    pb = bcps[bci % 2]; bt = bct[bci % 2]; bci += 1
                    for kc in range(8):
                        mm(pb[:], sBC[:, j, kc, :], a[:, kc, :], kc == 0, False)
                    mm(pb[:], ones_f[0:1, :], ar[:], False, True)
                    slot = {2: 0, 4: 1, 3: 2, 5: 3}[mi]
                    if mi == 4:
                        stt(bt[:], pb[:], 1.0, n2bc[:, half * 512:(half + 1) * 512], ALU.add, ALU.mult)
                    else:
                        cp('act', bt[:], pb[:])
                    dma(MODBC[slot, j, :, half * 512:(half + 1) * 512], bt[:])
        tt('dve', modsb[:], modps[:].rearrange("p (f q) -> p f q", q=4)[:, :, 0:3],
           abT[:].unsqueeze(2).broadcast_to([128, 48, 3]), ALU.add)
        stt(AB[:, 0, :, :], modsb[:, 8:16, :], 1.0, n1g[:].unsqueeze(2).broadcast_to([128, 8, 3]), ALU.add, ALU.mult)
        cp('dve', AB[:, 1, :, :], modsb[:, 0:8, :])
        stt(AB[:, 2, :, :], modsb[:, 32:40, :], 1.0, n2g[:].unsqueeze(2).broadcast_to([128, 8, 3]), ALU.add, ALU.mult)
        cp('dve', AB[:, 3, :, :], modsb[:, 24:32, :])
        dump("AB", AB[:], [128, 4, 8, 3])
    P.barrier()
    dump("modbc", MODBC[:, :, 0:1, :], [4, 2, 1, D])


    NCK = dict(allow_slow_non_contiguous=True)
    PI = math.pi
    if upto >= 2:
      with ExitStack() as st:
        lamT = sb(st, "lamT", [128, 2, 2, 32]); lst = sb(st, "lst", [128, 2, 32])
        bT = sb(st, "bT", [128, 2, 32, 16]); cT5 = sb(st, "cT5", [128, 2, 2, 512]); cn = sb(st, "cn", [128, 4, 128])
        dtt = sb(st, "dtt", [128, 2, 32]); lrd = sb(st, "lrd", [128, 2, 32]); th = sb(st, "th", [128, 2, 32])
        evi = sb(st, "evi", [128, 16], I32); evec = sb(st, "evec", [128, 16])
        PW = sb(st, "PW", [128, 2, 2, 32, 16])
        sm = [sb(st, f"sm{i}", [128, 2, 32]) for i in range(8)]
        fre = sb(st, "fre", [128, 2, 32]); fim = sb(st, "fim", [128, 2, 32])
        Bb = sb(st, "Bb", [128, 2, 2, 32, 16]); w1 = sb(st, "w1", [128, 2, 32, 16]); w2 = sb(st, "w2", [128, 2, 32, 16])
        A8 = sb(st, "A8", [128, 2, 2, 32])
        colTi = sb(st, "colTi", [128, 128], I32); colTf = sb(st, "colTf", [128, 128])
        rowSi = sb(st, "rowSi", [128, 1], I32); rowSf = sb(st, "rowSf", [128, 1])
        mTf = sb(st, "mTf", [128, 128]); mTb = sb(st, "mTb", [128, 128]); Dcol = sb(st, "Dcol", [128, 32])
        pct = ps(st, "pct", [128, 512]); ptp = [ps(st, f"ptp{i}", [128, 512]) for i in range(2)]
        pws = [ps(st, f"pws{i}", [128, 512]) for i in range(2)]
        st2 = ExitStack()
        arg2 = sb(st2, "arg2", [128, 2, 1024]); magl = sb(st2, "magl", [128, 1024]); mag = sb(st2, "mag", [128, 1024])
        nfi = sb(st2, "nfi", [128, 2, 1024], I32); nf = sb(st2, "nf", [128, 2, 1024]); rr = sb(st2, "rr", [128, 2, 1024])
        mk = sb(st2, "mk", [128, 2, 1024]); scs = sb(st2, "scs", [128, 2, 1024])
        for hf in range(2):
            hs = slice(hf * 64, hf * 64 + 64)
            for d_ in range(2):
                dma(lamT[hs, d_, 0, :], lam_re[d_].rearrange("g p -> p g"), **NCK)
                dma(lamT[hs, d_, 1, :], lam_im[d_].rearrange("g p -> p g"), **NCK)
            for ri in range(2):
                dma(bT[hs, ri, :, :], s5_b[ri].rearrange("g p h -> p g h"))
        for d_ in range(2):
            dma(lst[:, d_, :], log_step[d_].partition_broadcast(128).rearrange("p o g -> p (o g)"))
        for s_ in range(8):
            dma(Dcol[s_ * 16:(s_ + 1) * 16, :], s5_d.rearrange("o (g h) -> h (o g)", h=16), **NCK)
        for d_ in range(2):
            for ri in range(2):
                for hf in range(2):
                    dma(cn[:, :, hf * 64:(hf + 1) * 64], s5_c[d_][ri].rearrange("(rt r) p -> r rt p", r=128))
                for rt in range(4):
                    tr(pct[:, rt * 128:(rt + 1) * 128], cn[:, rt, :], ident_f[:])
                cp('dve', cT5[:, d_, ri, :], pct[:])
        iota(colTi[:].rearrange("p (t h) -> p t h", h=16), [[1, 8], [0, 16]], 0, 0)
        cp('dve', colTf[:], colTi[:])
        iota(rowSi[:], [[0, 1]], 0, 1)
        P.op('dve', lambda e: e.tensor_single_scalar(out=rowSi[:], in_=rowSi[:], scalar=4, op=ALU.arith_shift_right), [rowSi[:]], [rowSi[:]])
        cp('dve', rowSf[:], rowSi[:])
        ts('dve', mTf[:], colTf[:], rowSf[:, 0:1], None, ALU.is_ge)
        ts('dve', mTb[:], colTf[:], rowSf[:, 0:1], None, ALU.is_le)
        act(dtt[:], lst[:], AF.Exp)
        tt('dve', lrd[:], lamT[:, :, 0, :], dtt[:], ALU.mult)
        tt('dve', th[:], lamT[:, :, 1, :], dtt[:], ALU.mult)
        iota(evi[:], [[1, 16]], -7, 0)
        cp('dve', evec[:], evi[:])
        ev_b = evec[:].unsqueeze(1).broadcast_to([128, 64, 16])
        tt('dve', arg2[:, 0, :].rearrange("p (a e) -> p a e", e=16),
           th[:].rearrange("p d g -> p (d g)").unsqueeze(2).broadcast_to([128, 64, 16]), ev_b, ALU.mult)
        tt('dve', magl[:].rearrange("p (a e) -> p a e", e=16),
           lrd[:].rearrange("p d g -> p (d g)").unsqueeze(2).broadcast_to([128, 64, 16]), ev_b, ALU.mult)
        act(mag[:], magl[:], AF.Exp)
        ts('dve', arg2[:, 1, :], arg2[:, 0, :], PI / 2, None, ALU.add)
        ts('dve', nf[:], arg2[:], 1.0 / (2 * PI), None, ALU.mult)
        cp('dve', nfi[:], nf[:])
        cp('dve', nf[:], nfi[:])
        stt(rr[:], nf[:], -2 * PI, arg2[:], ALU.mult, ALU.add)
        ts('dve', mk[:], rr[:], PI, None, ALU.is_gt)
        stt(rr[:], mk[:], -2 * PI, rr[:], ALU.mult, ALU.add)
        ts('dve', mk[:], rr[:], -PI, None, ALU.is_lt)
        stt(rr[:], mk[:], 2 * PI, rr[:], ALU.mult, ALU.add)
        ts('dve', rr[:], rr[:], -3.141592, 3.141592, ALU.max, ALU.min)
        act(scs[:], rr[:], AF.Sin)
        tt('dve', PW[:, 0].rearrange("p d g e -> p (d g e)"), mag[:], scs[:, 1, :], ALU.mult)
        tt('dve', PW[:, 1].rearrange("p d g e -> p (d g e)"), mag[:], scs[:, 0, :], ALU.mult)
        P.barrier()
        st2.close()
        Lr = sb(st, "Lr", [128, 32, 8, 16]); Li = sb(st, "Li", [128, 32, 8, 16])
        Rr = sb(st, "Rr", [128, 32, 8, 16]); Ri = sb(st, "Ri", [128, 32, 8, 16])
        q1 = sb(st, "q1", [128, 32, 8, 16]); q2 = sb(st, "q2", [128, 32, 8, 16])
        T32 = sb(st, "T32", [128, 32, 128]); TOb = sb(st, "TOb", [128, 32, 128], BF16); tmpT = sb(st, "tmpT", [128, 4, 128])
        WS = sb(st, "WS", [128, 32, 2, 2, 64], BF16); WO = sb(st, "WO", [128, 32, 2, 128], BF16)
        are = PW[:, 0, :, :, 8]; aim = PW[:, 1, :, :, 8]
        lre = lamT[:, :, 0, :]; lim = lamT[:, :, 1, :]
        tt('dve', sm[0][:], lre, lre, ALU.mult)
        tt('dve', sm[1][:], lim, lim, ALU.mult)
        tt('dve', sm[0][:], sm[0][:], sm[1][:], ALU.add)
        recip(sm[1][:], sm[0][:])
        ts('dve', sm[2][:], are, -1.0, None, ALU.add)
        tt('dve', sm[3][:], sm[2][:], lre, ALU.mult)
        tt('dve', sm[4][:], aim, lim, ALU.mult)
        tt('dve', sm[3][:], sm[3][:], sm[4][:], ALU.add)
        tt('dve', fre[:], sm[3][:], sm[1][:], ALU.mult)
        tt('dve', sm[5][:], aim, lre, ALU.mult)
        tt('dve', sm[6][:], sm[2][:], lim, ALU.mult)
        tt('dve', sm[5][:], sm[5][:], sm[6][:], ALU.subtract)
        tt('dve', fim[:], sm[5][:], sm[1][:], ALU.mult)
        fre_b = fre[:].unsqueeze(3).broadcast_to([128, 2, 32, 16]); fim_b = fim[:].unsqueeze(3).broadcast_to([128, 2, 32, 16])
        bre_b = bT[:, 0].unsqueeze(1).broadcast_to([128, 2, 32, 16]); bim_b = bT[:, 1].unsqueeze(1).broadcast_to([128, 2, 32, 16])
        tt('dve', w1[:], fre_b, bre_b, ALU.mult); tt('dve', w2[:], fim_b, bim_b, ALU.mult)
        tt('dve', Bb[:, 0], w1[:], w2[:], ALU.subtract)
        tt('dve', w1[:], fre_b, bim_b, ALU.mult); tt('dve', w2[:], fim_b, bre_b, ALU.mult)
        tt('dve', Bb[:, 1], w1[:], w2[:], ALU.add)

        def esl(e0, step):
            i0 = e0 + 7
            if step == 1:
                return slice(i0, i0 + 8)
            stop = i0 - 8
            return slice(i0, stop if stop >= 0 else None, -1)

        def cprod(dre, dim_, Xre, Xim, d_, e0, step, neg_im):
            sl = esl(e0, step)
            Pre = PW[:, 0, d_, :, sl].unsqueeze(3).broadcast_to([128, 32, 8, 16])
            Pim = PW[:, 1, d_, :, sl].unsqueeze(3).broadcast_to([128, 32, 8, 16])
            Xr = Xre.unsqueeze(2).broadcast_to([128, 32, 8, 16]); Xi = Xim.unsqueeze(2).broadcast_to([128, 32, 8, 16])
            tt('dve', q1[:], Xr, Pre, ALU.mult); tt('pool', q2[:], Xi, Pim, ALU.mult)
            tt('dve', dre[:], q1[:], q2[:], ALU.subtract)
            tt('dve', q1[:], Xr, Pim, ALU.mult); tt('pool', q2[:], Xi, Pre, ALU.mult)
            if neg_im:
                stt(dim_[:], q1[:], -1.0, q2[:], ALU.mult, ALU.subtract)
            else:
                tt('dve', dim_[:], q1[:], q2[:], ALU.add)

        def c_of(d_, ri):
            return cT5[:, d_, ri, :].rearrange("p (g h) -> p g h", h=16)

        for d_ in range(2):
            Bre = Bb[:, 0, d_]; Bim = Bb[:, 1, d_]
            if d_ == 0:
                cprod(Lr, Li, Bre, Bim, 0, 0, -1, False); cprod(Rr, Ri, c_of(0, 0), c_of(0, 1), 0, 0, 1, True)
            else:
                cprod(Lr, Li, Bre, Bim, 1, 0, 1, False); cprod(Rr, Ri, c_of(1, 0), c_of(1, 1), 1, 0, -1, True)
            for gb in range(8):
                pt = ptp[gb % 2]
                for gi in range(4):
                    g = gb * 4 + gi
                    mm(pt[:, gi * 128:(gi + 1) * 128], Lr[0:64, g].rearrange("p s h -> p (s h)"),
                       Rr[0:64, g].rearrange("p s h -> p (s h)"), True, False)
                    mm(pt[:, gi * 128:(gi + 1) * 128], Li[0:64, g].rearrange("p s h -> p (s h)"),
                       Ri[0:64, g].rearrange("p s h -> p (s h)"), False, True)
                ptv = pt[:].rearrange("p (a c) -> p a c", a=4)
                if d_ == 0:
                    tt('dve', tmpT[:], ptv, mTf[:].unsqueeze(1).broadcast_to([128, 4, 128]), ALU.mult)
                    for gi in range(4):
                        g = gb * 4 + gi
                        stt(T32[:, g, :], ident_f[:], Dcol[:, g:g + 1], tmpT[:, gi, :], ALU.mult, ALU.add)
                else:
                    tt('dve', tmpT[:], ptv, mTb[:].unsqueeze(1).broadcast_to([128, 4, 128]), ALU.mult)
                    tt('dve', TOb[:, gb * 4:gb * 4 + 4, :], tmpT[:], T32[:, gb * 4:gb * 4 + 4, :], ALU.add)
            if d_ == 0:
                cprod(Lr, Li, Bre, Bim, 0, 7, -1, False)
            for ri, Lx in enumerate((Lr, Li)):
                for gb in range(4):
                    pw = pws[gb % 2]
                    for gi in range(8):
                        g = gb * 8 + gi
                        tr(pw[:, gi * 64:(gi + 1) * 64], Lx[0:64, g].rearrange("p s h -> p (s h)"), ident_f[0:64, 0:64])
                    cp('act', WS[:, gb * 8:gb * 8 + 8, d_, ri, :], pw[:].rearrange("p (a c) -> p a c", a=8))
            if d_ == 0:
                cprod(Rr, Ri, c_of(0, 0), c_of(0, 1), 0, 1, 1, True)
            else:
                cprod(Rr, Ri, c_of(1, 0), c_of(1, 1), 1, 8, -1, True)
            hs = slice(d_ * 64, d_ * 64 + 64)
            cp('act', WO[hs, :, 0, :], Rr[hs].rearrange("p g s h -> p g (s h)"))
            cp('act', WO[hs, :, 1, :], Ri[hs].rearrange("p g s h -> p g (s h)"))
            cp('dve', A8[hs, 0, 0, :], PW[hs, 0, d_, :, 15]); cp('dve', A8[hs, 0, 1, :], PW[hs, 0, d_, :, 15])
            ts('dve', A8[hs, 1, 0, :], PW[hs, 1, d_, :, 15], -1.0, None, ALU.mult)
            cp('dve', A8[hs, 1, 1, :], PW[hs, 1, d_, :, 15])
        dma(TOEP_S, TOb[:].rearrange("p g c -> p (g c)"))
        dma(WS_S, WS[:].rearrange("p g d r c -> p (g d r c)"))
        dma(WO_S, WO[:].rearrange("p g r c -> p (g r c)"))
        dma(A8_S, A8[:].rearrange("p a r g -> p (a r g)"))
        dump("TOb", TOb[:], [128, 32, 128], BF16)
        dump("PW", PW[:], [128, 2, 2, 32, 16])
        dump("Bb", Bb[:], [128, 2, 2, 32, 16])
      P.barrier()

    def reduce_sum(o, i):
        return P.op('dve', lambda e, o=o, i=i: e.reduce_sum(out=o, in_=i, axis=AX.X), [i], [o])

    NBLK = 2 * NE
    cum = sb(es, "cum", [128, NE]); IDX = sb(es, "IDX", [128, 2 * NSET * 8], U32)
    ecol = sb(es, "ecol", [128, NE]); ecoli = sb(es, "ecoli", [128, NE], I32); ecol0 = sb(es, "ecol0", [128, NE])
    V8A = sb(es, "V8A", [128, 2 * NSET * 8]); W8A = sb(es, "W8A", [128, 2 * NSET * 8])
    RIA = sb(es, "RIA", [128, 2 * NSET * 8, 2], I32); EBI = sb(es, "EBI", [128, 2], I32); EBW = sb(es, "EBW", [128, NE], U32)
    if upto >= 4:
        with ExitStack() as st:
            zt = sb(st, "zt", [128, NBLK * 2], I32); ztb = sb(st, "ztb", [1, D], BF16)
            memset('dve', zt[:], 0)
            memset('dve', ztb[:], 0.0)
            memset('dve', cum[:], 0.0)
            iota(ecoli[:], [[1, NE]], 1, 0)
            cp('dve', ecol[:], ecoli[:])
            ts('dve', ecol0[:], ecol[:], -1.0, None, ALU.add)
            dma(RINFO[1:NROW + 1, :].rearrange("(p q) t -> p (q t)", p=128), zt[:])
            dma(RINFO[0:1, :], zt[0:1, 0:2])
            dma(Y_S[0:1, :], ztb[:])
        P.barrier()

    for b in range(2):
        if upto < 1:
            break
        seqst = ExitStack()
        mixg = sb(seqst, f"mixg{b}", [128, 4, T], BF16)
        with ExitStack() as st:
            Wi = sb(st, "Wi", [128, 8, DIN], BF16)
            wa = sb(st, "wa", [16, 2, 256]); ba = sb(st, "ba", [1, 2, 256]); gng = sb(st, "gng", [128, 128])
            OF = sb(st, "OF", [128, NT, 512], BF16)
            xts = [sb(st, f"xt{i}", [128, D]) for i in range(2)]
            junk = sb(st, "junk", [128, D], BF16); xn = sb(st, "xn", [128, D], BF16)
            ss = sb(st, "ss", [128, 1]); rstd = sb(st, "rstd", [128, 1]); tmpc = sb(st, "tmpc", [128, 1])
            hT = sb(st, "hT", [128, 8, 128], BF16)
            lrT = sb(st, "lrT", [16, 128]); e1 = sb(st, "e1", [128, 256]); lsp = sb(st, "lsp", [128, 256])
            kdec = sb(st, "kdec", [128, 256]); kS = sb(st, "kS", [128, 256], BF16)
            Ep = sb(st, "Ep", [128, 256]); Em = sb(st, "Em", [128, 256])
            qc = sb(st, "qc", [128, 256], BF16); kI = sb(st, "kI", [128, 256], BF16)
            vb = sb(st, "vb", [128, 512], BF16); ub = sb(st, "ub", [128, 512], BF16)
            sTm = sb(st, "sTm", [128, 512], BF16)
            S32 = sb(st, "S32", [128, 2, 128]); Sbf = sb(st, "Sbf", [128, 2, 128], BF16)
            ot = sb(st, "ot", [128, 512]); osq = sb(st, "osq", [128, 512]); on = sb(st, "on", [128, 512])
            sg = sb(st, "sg", [128, 512]); go = sb(st, "go", [128, 512], BF16)
            ssh = sb(st, "ssh", [128, 4]); rsh = sb(st, "rsh", [128, 4]); tmp4 = sb(st, "tmp4", [128, 4])
            pT = ps(st, "pT", [128, 1024], BF16); pq = ps(st, "pq", [128, 512]); pkz = ps(st, "pkz", [128, 512])
            pk = pkz[:, 0:256]; pz = pkz[:, 256:512]; pv = ps(st, "pv", [128, 512]); pgu = ps(st, "pgu", [128, 512])
            pDc = ps(st, "pDc", [128, 512]); pD = pDc[:, 0:256]; pc = pDc[:, 256:512]; pss = ps(st, "pss", [128, 512])
            po = ps(st, "po", [128, 512])
            dma(Wi[:], WIN_BF.rearrange("(kc p) n -> p kc n", p=128))
            for d_ in range(2):
                dma(wa[:, d_, :], gla_wa[d_])
                dma(ba[:, d_, :], gla_ba[d_])
            dma(gng[:], gla_norm_g.partition_broadcast(128).rearrange("p o d -> p (o d)"))
            tcount = [0]

            xpend = {}

            def gla_tile(src, usrc, n, dirn, want_out, first_pass, nxt=None):
                jm = b if want_out else 2
                key = (dirn, want_out, n)
                if key not in xpend:
                    xpend[key] = xts[tcount[0] % 2]; tcount[0] += 1
                    dma(xpend[key][:], src[b, n * 128:(n + 1) * 128, :])
                xt = xpend.pop(key)
                if nxt is not None:
                    nsrc, nkey = nxt
                    xpend[nkey] = xts[tcount[0] % 2]; tcount[0] += 1
                    dma(xpend[nkey][:], nsrc[b, nkey[2] * 128:(nkey[2] + 1) * 128, :])
                act(junk[:], xt[:], AF.Square, accum=ss[:])
                rsqrt_col(rstd[:], ss[:], 1.0 / D, tmpc[:])
                ts('dve', xn[:], xt[:], rstd[:, 0:1], None, ALU.mult)
                for kc in range(8):
                    tr(pT[:, kc * 128:(kc + 1) * 128], xn[:, kc * 128:(kc + 1) * 128], ident_b[:])
                for kc in range(8):
                    if kc % 2 == 0:
                        ts('dve', hT[:, kc, :], pT[:, kc * 128:(kc + 1) * 128], AB[:, 0, kc, jm:jm + 1],
                           AB[:, 1, kc, jm:jm + 1], ALU.mult, ALU.add)
                    else:
                        act(hT[:, kc, :], pT[:, kc * 128:(kc + 1) * 128], AF.Identity,
                            bias=AB[:, 1, kc, jm:jm + 1], scale=AB[:, 0, kc, jm:jm + 1])
                for gi in range(4):
                    for kc in range(8):
                        mm(pq[:, gi * 128:(gi + 1) * 128], Wi[:, kc, gi * 128:(gi + 1) * 128], hT[:, kc, :], kc == 0, kc == 7)
                for kc in range(8):
                    mm(pk[:], hT[:, kc, :], Wi[:, kc, 256:512], kc == 0, kc == 7)
                for kc in range(8):
                    mm(pv[:], hT[:, kc, :], Wi[:, kc, 512:1024], kc == 0, kc == 7)
                c0 = 1536 + 16 * dirn
                for kc in range(8):
                    mm(pc[0:16, 0:128], Wi[:, kc, c0:c0 + 16], hT[:, kc, :], kc == 0, kc == 7)
                cp('act', lrT[:], pc[0:16, 0:128])
                if first_pass:
                    for kc in range(8):
                        mm(pgu[:], hT[:, kc, :], Wi[:, kc, 1568:2080], kc == 0, kc == 7)
                    cp('act', ub[:], pgu[:])
                    dma(usrc[b, n * 128:(n + 1) * 128, :], ub[:])
                elif want_out:
                    for kc in range(8):
                        mm(pgu[:], hT[:, kc, :], Wi[:, kc, 1024:1536], kc == 0, kc == 7)
                mm(pz[:], lrT[:], wa[:, dirn, :], True, False)
                mm(pz[:], ones_f[0:1, :], ba[:, dirn, :], False, True)
                act(e1[:], pz[:], AF.Exp, scale=-1.0)
                act(lsp[:], e1[:], AF.Ln, bias=1.0)
                mm(pD[:], (SLm if dirn == 0 else SUm)[:], lsp[:])
                act(kdec[:], pD[:], AF.Exp, scale=-1.0 / 16)
                tt('dve', kS[:], pk[:], kdec[:], ALU.mult)
                msk = maskF if dirn == 0 else maskB
                for hp in range(2):
                    mm(pc[:, hp * 128:(hp + 1) * 128], lsp[:, hp * 128:(hp + 1) * 128], msk[:])
                act(Ep[:], pc[:], AF.Exp, scale=-1.0 / 16)
                act(Em[:], pc[:], AF.Exp, scale=1.0 / 16)
                stt(qc[:], pq[:, 0:256], 0.125, Ep[:], ALU.mult, ALU.mult)
                tt('dve', kI[:], pq[:, 256:512], Em[:], ALU.mult)
                cp('act', vb[:], pv[:])
                last = 127 if dirn == 0 else 0
                for h in range(4):
                    hp = h // 2; r0 = (h % 2) * 64
                    mm(pss[:, h * 128:(h + 1) * 128], kI[r0:r0 + 64, hp * 128:(hp + 1) * 128],
                       qc[r0:r0 + 64, hp * 128:(hp + 1) * 128])
                tt('dve', sTm[:].rearrange("p (h c) -> p h c", h=4), pss[:].rearrange("p (h c) -> p h c", h=4),
                   msk[:].unsqueeze(1).broadcast_to([128, 4, 128]), ALU.mult)
                if want_out:
                    for h in range(4):
                        hp = h // 2; r0 = (h % 2) * 64
                        mm(po[:, h * 128:(h + 1) * 128], sTm[:, h * 128:(h + 1) * 128], vb[:, h * 128:(h + 1) * 128], True, False)
                        mm(po[:, h * 128:(h + 1) * 128], qc[r0:r0 + 64, hp * 128:(hp + 1) * 128], Sbf[r0:r0 + 64, hp, :], False, True)
                for h in range(4):
                    hp = h // 2; r0 = (h % 2) * 64
                    mm(pD[r0:r0 + 64, hp * 128:(hp + 1) * 128], kS[:, h * 64:(h + 1) * 64], vb[:, h * 128:(h + 1) * 128])
                for hp in range(2):
                    stt(S32[:, hp, :], S32[:, hp, :], Ep[:, hp * 128 + last:hp * 128 + last + 1],
                        pD[:, hp * 128:(hp + 1) * 128], ALU.mult, ALU.add)
                cp('act', Sbf[:], S32[:])
                if want_out and first_pass:
                    cp('act', OF[:, n, :], po[:])
                if want_out and not first_pass:
                    tt('dve', ot[:], po[:], OF[:, n, :], ALU.add)
                    tt('dve', osq[:], ot[:], ot[:], ALU.mult)
                    reduce_sum(ssh[:], osq[:].rearrange("p (h c) -> p h c", h=4))
                    rsqrt_col(rsh[:], ssh[:], 1.0 / 128, tmp4[:])
                    tt('dve', on[:].rearrange("p (h c) -> p h c", h=4), ot[:].rearrange("p (h c) -> p h c", h=4),
                       rsh[:].unsqueeze(2).broadcast_to([128, 4, 128]), ALU.mult)
                    tt('dve', on[:].rearrange("p (h c) -> p h c", h=4), on[:].rearrange("p (h c) -> p h c", h=4),
                       gng[:].unsqueeze(1).broadcast_to([128, 4, 128]), ALU.mult)
                    act(sg[:], pgu[:], AF.Silu)
                    tt('dve', go[:], on[:], sg[:], ALU.mult)
                    for k4 in range(4):
                        tr(pT[:, k4 * 128:(k4 + 1) * 128], go[:, k4 * 128:(k4 + 1) * 128], ident_b[:])
                    cp('act', mixg[:, :, n * 128:(n + 1) * 128], pT[:, 0:512].rearrange("p (k c) -> p k c", k=4))

            for dirn in range(2):
                memset('dve', S32[:], 0.0)
                memset('dve', Sbf[:], 0.0)
                order_c = list(range(NCT)) if dirn == 0 else list(reversed(range(NCT)))
                order_x = list(range(NT)) if dirn == 0 else list(reversed(range(NT)))
                seq_ = [(ctx, UC_S, n, False) for n in order_c] + [(x, U_S, n, True) for n in order_x]
                for qi_, (src_, us_, n_, wo_) in enumerate(seq_):
                    nx_ = None
                    if qi_ + 1 < len(seq_):
                        nx_ = (seq_[qi_ + 1][0], (dirn, seq_[qi_ + 1][3], seq_[qi_ + 1][2]))
                    gla_tile(src_, us_, n_, dirn, wo_, dirn == 0, nxt=nx_)
            if b == 0:
                dump("mixg", mixg[:], [128, 4, T], BF16)
        P.barrier()
        if upto < 3:
            seqst.close()
            continue
        mixs = sb(seqst, f"mixs{b}", [128, 4, NSET, 128], BF16)
        with ExitStack() as st:
            TOw = sb(st, "TOw", [128, 32, 128], BF16); WSw = sb(st, "WSw", [128, 32, 2, 2, 64], BF16)
            WOw = sb(st, "WOw", [128, 32, 2, 128], BF16); A8w = sb(st, "A8w", [128, 2, 2, 32])
            GLw = sb(st, "GLw", [128, 4, 512], BF16); glb = sb(st, "glb", [128, 4])
            UF = [sb(st, f"UF{g}", [128, NK], BF16) for g in range(32)]
            XH = sb(st, "XH", [128, NK, 2, 32], BF16)
            Xs = [sb(st, f"Xs{i}", [128, 2, 32]) for i in range(2)]
            s1 = sb(st, "s1", [128, 2, 32]); s2 = sb(st, "s2", [128, 2, 32])
            stu = ExitStack()
            Ucm = sb(stu, "Ucm", [128, NCTL, 32, 128], BF16); Ucc = sb(stu, "Ucc", [NKC, 32, 128], BF16); Ustg = sb(stu, "Ustg", [128, 4096], BF16)
            pUF = ps(st, "pUF", [128, 1024], BF16); pSI = [ps(st, f"pSI{i}", [128, 512]) for i in range(2)]
            py = ps(st, "py", [128, 512]); pT2 = ps(st, "pT2", [128, 1024], BF16)
            pgl = [ps(st, f"pgl{i}", [128, 512]) for i in range(2)]
            dma(TOw[:].rearrange("p g c -> p (g c)"), TOEP_S)
            dma(WSw[:].rearrange("p g d r c -> p (g d r c)"), WS_S)
            dma(WOw[:].rearrange("p g r c -> p (g r c)"), WO_S)
            dma(A8w[:].rearrange("p a r g -> p (a r g)"), A8_S)
            dma(GLw[:], GLU_BF.rearrange("(kc p) n -> p kc n", p=128))
            dma(glb[:], s5_glu_b.rearrange("o (kc p) -> p (o kc)", p=128), **NCK)
            for ct in range(NCTL):
                dma(Ustg[0:CPT], U_S[b, ct * 8 * CPT:(ct + 1) * 8 * CPT, :].rearrange("(c j) ch -> c (j ch)", j=8))
                cp('dve', Ucm[0:CPT, ct].rearrange("c g (j h) -> c g j h", j=8),
                   Ustg[0:CPT].rearrange("c (j g h) -> c g j h", j=8, g=32))
            dma(Ustg[0:NKC], UC_S[b].rearrange("(c j) ch -> c (j ch)", j=8))
            cp('dve', Ucc[:].rearrange("c g (j h) -> c g j h", j=8), Ustg[0:NKC].rearrange("c (j g h) -> c g j h", j=8, g=32))
            for g in range(32):
                tr(pUF[:, 0:NKC], Ucc[:, g, :], ident_b[0:NKC, 0:NKC])
                for ct in range(NCTL):
                    tr(pUF[:, NKC + ct * CPT:NKC + (ct + 1) * CPT], Ucm[0:CPT, ct, g, :], ident_b[0:CPT, 0:CPT])
                cp('dve', UF[g][:], pUF[:, 0:NK])
                for ri in range(2):
                    for d_ in range(2):
                        mm(pSI[ri][d_ * 64:(d_ + 1) * 64, 0:NK], WSw[:, g, d_, ri, :], UF[g][:])
                    eng = 'act' if ri == 0 else 'dve'
                    cp(eng, XH[0:64, :, ri, g], pSI[ri][0:64, 0:NK])
                    cp(eng, XH[64:128, 0:NKC, ri, g], pSI[ri][64:128, 0:NKC][:, ::-1])
                    cp(eng, XH[64:128, NKC:NK, ri, g], pSI[ri][64:128, NKC:NK][:, ::-1])
            P.barrier()
            stu.close()
            XHN = sb(st, "XHN", [128, NK, 2, 32], BF16)
            zc = sb(st, "zc", [128, 4, 8, 8, 16], BF16)
            gx2 = sb(st, "gx2", [128, 512]); gin = sb(st, "gin", [128, 512]); gsg = sb(st, "gsg", [128, 512])
            sgl = [sb(st, f"sgl{i}", [128, 512], BF16) for i in range(4)]
            memset('dve', Xs[0][:], 0.0)
            AR2 = A8w[:, 0]; AI2 = A8w[:, 1]
            for k in range(NK):
                Xp = Xs[k % 2]; Xn = Xs[(k + 1) % 2]
                tt('dve', s1[:], AR2, Xp[:], ALU.mult)
                tt('dve', s2[:], AI2, Xp[:, ::-1, :], ALU.mult)
                tt('dve', s1[:], s1[:], s2[:], ALU.add)
                tt('dve', Xn[:], s1[:], (XH[:, k], k), ALU.add)
                cp('act', (XH[:, k], k), Xn[:])
            P.barrier()
            nq = 4
            for qi_ in range(nq):
                a0 = qi_ * NK // nq; a1 = (qi_ + 1) * NK // nq
                cp(['dve', 'pool', 'act', 'dve'][qi_], XHN[64:128, a0:a1], XH[64:128, NK - 1 - a0:(NK - 1 - a1 if NK - 1 - a1 >= 0 else None):-1])
            for ct in range(NCTL):
                kf0 = NKC - 1 + ct * CPT
                kb0 = NK - 2 - ct * CPT
                for gb in range(8):
                    for gi in range(4):
                        g = gb * 4 + gi
                        o_ = py[0:CPT, gi * 128:(gi + 1) * 128]
                        mm(o_, UF[g][:, NKC + ct * CPT:NKC + (ct + 1) * CPT], TOw[:, g, :], True, False)
                        for ri in range(2):
                            mm(o_, XH[0:64, kf0:kf0 + CPT, ri, g], WOw[0:64, g, ri, :], False, False)
                        for ri in range(2):
                            lb = XHN[64:128, ct * CPT + 1:ct * CPT + 1 + CPT, ri, g]
                            mm(o_, lb, WOw[64:128, g, ri, :], False, ri == 1)
                    yv = py[0:CPT, :]
                    act(gx2[0:CPT], yv, AF.Square)
                    ts('dve', gx2[0:CPT], gx2[0:CPT], 0.044715, 1.0, ALU.mult, ALU.add)
                    tt('dve', gin[0:CPT], gx2[0:CPT], yv, ALU.mult)
                    act(gsg[0:CPT], gin[0:CPT], AF.Sigmoid, scale=1.5957691216057308)
                    tt('dve', zc[0:CPT, gb // 2, :, (gb % 2) * 4:(gb % 2) * 4 + 4, :].rearrange("c j g h -> c g j h"),
                       gsg[0:CPT].rearrange("c (g j h) -> c g j h", g=4, j=8), yv.rearrange("c (g j h) -> c g j h", g=4, j=8), ALU.mult)
                for kt in range(4):
                    for j in range(8):
                        tr(pT2[:, j * 128:j * 128 + CPT], zc[0:CPT, kt, j].rearrange("c g h -> c (g h)"), ident_b[0:CPT, 0:CPT])
                    cp('act', mixs[:, kt, ct * 8:(ct + 1) * 8, 0:CPT], pT2[:].rearrange("p (j c) -> p j c", j=8)[:, :, 0:CPT])
            for s0 in range(0, NSET, 4):
                for oc in range(4):
                    pg_ = pgl[oc % 2]
                    for kt in range(4):
                        mm(pg_[:], GLw[:, kt, oc * 128:(oc + 1) * 128], mixs[:, kt, s0:s0 + 4, :].rearrange("p s c -> p (s c)"), kt == 0, kt == 3)
                    act(sgl[oc][:], pg_[:], AF.Sigmoid, bias=glb[:, oc:oc + 1])
                for oc in range(4):
                    mv = mixs[:, oc, s0:s0 + 4, :].rearrange("p s c -> p (s c)")
                    tt('dve', mv, mv, sgl[oc][:], ALU.mult)
            if b == 0:
                dump("mixs", mixs[:], [128, 4, NSET, 128], BF16)
        P.barrier()
        if upto >= 4:
          with ExitStack() as st:
            WOb = sb(st, "WOb", [128, 8, D], BF16); RWb = sb(st, "RWb", [128, 8, NE], BF16)
            SGb = sb(st, "SGb", [128, 8, 256], BF16); SUb = sb(st, "SUb", [128, 8, 256], BF16); SDb = sb(st, "SDb", [128, 2, D], BF16)
            G1 = sb(st, "G1", [128, D]); A2bc = sb(st, "A2bc", [128, D]); B2bc = sb(st, "B2bc", [128, D]); G2 = sb(st, "G2", [128, D])
            rbbc = sb(st, "rbbc", [128, NE])
            xts4 = [sb(st, f"xt4{i}", [128, D]) for i in range(2)]
            t1k = sb(st, "t1k", [128, D]); x1 = sb(st, "x1", [128, D]); x1p = sb(st, "x1p", [128, D])
            junk4 = sb(st, "junk4", [128, D], BF16); hx2 = sb(st, "hx2", [128, D], BF16); hx2T = sb(st, "hx2T", [128, 8, 128], BF16)
            ss4 = sb(st, "ss4", [128, 1]); rstd4 = sb(st, "rstd4", [128, 1]); tmpc4 = sb(st, "tmpc4", [128, 1])
            R_ = {n: sb(st, "r_" + n, [128, NE]) for n in ["scores", "biased", "masked", "sel", "selw", "Wd", "pos", "val", "vm", "hi", "pm", "jk"]}
            selb = sb(st, "selb", [128, NE], BF16)
            m8 = sb(st, "m8", [128, 8, 8]); gs = sb(st, "gs", [128, 8]); gm = sb(st, "gm", [128, 8]); gmask = sb(st, "gmask", [128, 8])
            pen = sb(st, "pen", [128, 8]); t8 = sb(st, "t8", [128, 8]); v8 = sb(st, "v8", [128, 8]); w8 = sb(st, "w8", [128, 8])
            den = sb(st, "den", [128, 1]); rden = sb(st, "rden", [128, 1])
            sgate = sb(st, "sgate", [128, 256]); hidT = sb(st, "hidT", [128, 256], BF16)
            p1 = [ps(st, f"p1{i}", [128, 512]) for i in range(2)]
            pT4 = ps(st, "pT4", [128, 1024], BF16); pl = ps(st, "pl", [128, 512]); ptot = ps(st, "ptot", [128, 512])
            pgu2 = ps(st, "pgu2", [128, 512]); psh = [ps(st, f"psh{i}", [128, 512]) for i in range(2)]
            dma(WOb[:], WOUT_BF.rearrange("(kc p) n -> p kc n", p=128))
            dma(RWb[:], RW_BF.rearrange("(kc p) n -> p kc n", p=128))
            dma(SGb[:], SHG_BF.rearrange("(kc p) n -> p kc n", p=128))
            dma(SUb[:], SHU_BF.rearrange("(kc p) n -> p kc n", p=128))
            dma(SDb[:], SHD_BF.rearrange("(kc p) n -> p kc n", p=128))
            dma(G1[:], MODBC[0, b]); dma(A2bc[:], MODBC[1, b]); dma(B2bc[:], MODBC[2, b]); dma(G2[:], MODBC[3, b])
            dma(rbbc[:], router_b.partition_broadcast(128).rearrange("p o d -> p (o d)"))

            def vmax(o, i):
                return P.op('dve', lambda e, o=o, i=i: e.max(out=o, in_=i), [i], [o])

            for sidx in range(NSET):
                ct = sidx // 8; j = sidx % 8
                gset = b * NSET + sidx
                r0 = ct * 1024 + j
                rsl = slice(r0, r0 + 8 * (CPT - 1) + 1, 8)
                xt = xts4[sidx % 2]
                if sidx == 0:
                    dma(xt[:], x[b, rsl, :])
                if sidx + 1 < NSET:
                    r1_ = ((sidx + 1) // 8) * 1024 + (sidx + 1) % 8
                    dma(xts4[(sidx + 1) % 2][:], x[b, r1_:r1_ + 8 * (CPT - 1) + 1:8, :])
                for half in range(2):
                    for kc in range(8):
                        lt = mixg[:, kc, rsl] if kc < 4 else mixs[:, kc - 4, sidx, :]
                        mm(p1[half][:], lt, WOb[:, kc, half * 512:(half + 1) * 512], kc == 0, kc == 7)
                for half in range(2):
                    hs_ = slice(half * 512, (half + 1) * 512)
                    tt('dve', t1k[:, hs_], p1[half][:], G1[:, hs_], ALU.mult)
                tt('dve', x1[:], t1k[:], xt[:], ALU.add)
                act(junk4[:], x1[:], AF.Square, accum=ss4[:])
                rsqrt_col(rstd4[:], ss4[:], 1.0 / D, tmpc4[:])
                stt(t1k[:], x1[:], rstd4[:, 0:1], A2bc[:], ALU.mult, ALU.mult)
                tt('dve', hx2[:], t1k[:], B2bc[:], ALU.add)
                dma(HX2_S[b * T + r0:b * T + r0 + 8 * (CPT - 1) + 1:8, :], hx2[:])
                for kc in range(8):
                    tr(pT4[:, kc * 128:(kc + 1) * 128], hx2[:, kc * 128:(kc + 1) * 128], ident_b[:])
                cp('act', hx2T[:].rearrange("p k c -> p (k c)"), pT4[:])
                for kc in range(8):
                    mm(pl[:, 0:NE], hx2T[:, kc, :], RWb[:, kc, :], kc == 0, kc == 7)
                act(R_["scores"][:], pl[:, 0:NE], AF.Sigmoid)
                tt('dve', R_["biased"][:], R_["scores"][:], rbbc[:], ALU.add)
                for g8 in range(8):
                    vmax(m8[:, g8, :], R_["biased"][:, g8 * 32:(g8 + 1) * 32])
                tt('dve', gs[:], m8[:, :, 0], m8[:, :, 1], ALU.add)
                vmax(gm[:], gs[:])
                ts('dve', gmask[:], gs[:], gm[:, 3:4], None, ALU.is_ge)
                ts('dve', pen[:], gmask[:], -1.0, 1e9, ALU.add, ALU.mult)
                tt('dve', R_["masked"][:].rearrange("p (g e) -> p g e", g=8), R_["biased"][:].rearrange("p (g e) -> p g e", g=8),
                   pen[:].unsqueeze(2).broadcast_to([128, 8, 32]), ALU.add)
                vmax(t8[:], R_["masked"][:])
                ts('dve', R_["sel"][:], R_["masked"][:], t8[:, 7:8], None, ALU.is_ge)
                tt('dve', R_["selw"][:], R_["sel"][:], R_["scores"][:], ALU.mult)
                reduce_sum(den[:], R_["selw"][:])
                recip(rden[:], den[:])
                ts('dve', R_["Wd"][:], R_["selw"][:], rden[:, 0:1], 2.5, ALU.mult, ALU.mult)
                cp('dve', selb[:], R_["sel"][:])
                mm(pl[:, NE:2 * NE], SU_b[:], selb[:])
                mm(ptot[:, 0:NE], ones_b[:], selb[:])
                tt('dve', R_["pos"][:], pl[:, NE:2 * NE], cum[:], ALU.add)
                tt('dve', cum[:], cum[:], ptot[:, 0:NE], ALU.add)
                stt(R_["val"][:], R_["pos"][:], float(NE), ecol[:], ALU.mult, ALU.add)
                tt('dve', R_["val"][:], R_["val"][:], R_["sel"][:], ALU.mult)
                vmax(v8[:], R_["val"][:])
                for k in range(8):
                    stt(R_["jk"][:], R_["val"][:], v8[:, k:k + 1], R_["Wd"][:], ALU.is_equal, ALU.mult, accum=w8[:, k:k + 1])
                cp('dve', V8A[:, gset * 8:(gset + 1) * 8], v8[:])
                cp('dve', W8A[:, gset * 8:(gset + 1) * 8], w8[:])
                for oc in range(2):
                    for kc in range(8):
                        mm(pgu2[:, oc * 128:(oc + 1) * 128], SGb[:, kc, oc * 128:(oc + 1) * 128], hx2T[:, kc, :], kc == 0, kc == 7)
                for oc in range(2):
                    for kc in range(8):
                        mm(pgu2[:, 256 + oc * 128:256 + (oc + 1) * 128], SUb[:, kc, oc * 128:(oc + 1) * 128], hx2T[:, kc, :], kc == 0, kc == 7)
                act(sgate[:], pgu2[:, 0:256], AF.Silu)
                tt('dve', hidT[:], sgate[:], pgu2[:, 256:512], ALU.mult)
                for half in range(2):
                    for oc in range(2):
                        mm(psh[half][:], hidT[:, oc * 128:(oc + 1) * 128], SDb[:, oc, half * 512:(half + 1) * 512], oc == 0, oc == 1)
                for half in range(2):
                    hs_ = slice(half * 512, (half + 1) * 512)
                    tt('dve', t1k[:, hs_], psh[half][:], G2[:, hs_], ALU.mult)
                tt('dve', x1p[:], t1k[:], x1[:], ALU.add)
                dma(X1_S[b, rsl, :], x1p[:])
                if b == 0 and sidx == 6:
                    dump("pos6", R_["pos"][:], [128, NE]); dump("sel6", R_["sel"][:], [128, NE]); dump("cum6", cum[:], [128, NE]); dump("vm6", R_["vm"][:], [128, NE]); dump("val6", R_["val"][:], [128, NE])
                if b == 0 and sidx == 0:
                    dump("x1", x1[:], [128, D]); dump("Wd", R_["Wd"][:], [128, NE]); dump("val", R_["val"][:], [128, NE])
                    dump("x1p", x1p[:], [128, D]); dump("w8", w8[:], [128, 8]); dump("v8", v8[:], [128, 8]); dump("sel", R_["sel"][:], [128, NE]); dump("scores", R_["scores"][:], [128, NE]); dump("pos", R_["pos"][:], [128, NE])
        P.barrier()
        seqst.close()
        P.barrier()


    NSA = 2 * NSET * 8
    if upto >= 5:
        with ExitStack() as st:
            c128 = sb(st, "c128", [128, NE]); ovb = sb(st, "ovb", [128, NE]); cs = sb(st, "cs", [128, NE]); obs = sb(st, "obs", [128, NE])
            tiB = sb(st, "tiB", [128, NE], I32); jkB = sb(st, "jkB", [128, NE]); jcol = sb(st, "jcol", [128, 2]); jcoli = sb(st, "jcoli", [128, 2], I32)
            ebf = sb(st, "ebf", [128, 2])
            vf = sb(st, "vf", [128, NSA]); vi = sb(st, "vi", [128, NSA], I32); posi = sb(st, "posi", [128, NSA], I32); ei = sb(st, "ei", [128, NSA], I32)
            ef = sb(st, "ef", [128, NSA]); posf = sb(st, "posf", [128, NSA]); obs8 = sb(st, "obs8", [128, NSA])
            isov = sb(st, "isov", [128, NSA]); qf = sb(st, "qf", [128, NSA]); qi = sb(st, "qi", [128, NSA], I32); q2 = sb(st, "q2i", [128, NSA], I32)
            pov = sb(st, "pov", [128, NSA]); bov = sb(st, "bov", [128, NSA]); Rf = sb(st, "Rf", [128, NSA])

            def tss(o, i, sc, op):
                return P.op('dve', lambda e, o=o, i=i, sc=sc, op=op: e.tensor_single_scalar(out=o, in_=i, scalar=sc, op=op), [i], [o])

            ts('dve', c128[:], cum[:], -128.0, 0.0, ALU.add, ALU.max)
            ts('dve', c128[:], c128[:], 127.0, None, ALU.add)
            cp('dve', tiB[:], c128[:])
            tss(tiB[:], tiB[:], 7, ALU.arith_shift_right)
            cp('dve', ovb[:], tiB[:])
            P.op('dve', lambda e: e.tensor_tensor_scan(out=cs[:], data0=ones_f[:, 0:1].broadcast_to([128, NE]), data1=ovb[:], initial=0.0, op0=ALU.mult, op1=ALU.add), [ovb[:], ones_f[:]], [cs[:]])
            tt('dve', obs[:], cs[:], ovb[:], ALU.subtract)
            iota(jcoli[:], [[128, 2]], 0, 1)
            cp('dve', jcol[:], jcoli[:])
            for h in range(2):
                ts('dve', jkB[:], cs[:], jcol[:, h:h + 1], None, ALU.is_le)
                reduce_sum(ebf[:, h:h + 1], jkB[:])
            cp('dve', EBI[:], ebf[:])
            with ExitStack() as st3:
                dg = sb(st3, "dg", [128, 128]); ebrow = sb(st3, "ebrow", [128, NE]); pcol = sb(st3, "pcol", [128, 1]); pcoli = sb(st3, "pcoli", [128, 1], I32)
                peb = ps(st3, "peb", [128, 512])
                for h in range(2):
                    ts('dve', dg[:], ident_f[:], ebf[:, h:h + 1], None, ALU.mult)
                    mm(peb[:, h * 128:(h + 1) * 128], ones_f[:], dg[:])
                iota(pcoli[:], [[0, 1]], 0, 1)
                cp('dve', pcol[:], pcoli[:])
                ts('dve', ebrow[:], peb[:, 0:NE], 128.0, pcol[:, 0:1], ALU.mult, ALU.add)
                cp('dve', EBW[:], ebrow[:])
                dump("EBW", EBW[:], [128, NE], U32)
            ts('dve', vf[:], V8A[:], -1.0, None, ALU.add)
            cp('dve', vi[:], vf[:])
            tss(posi[:], vi[:], 8, ALU.arith_shift_right)
            tss(ei[:], vi[:], 255, ALU.bitwise_and)
            cp('dve', ef[:], ei[:]); cp('dve', posf[:], posi[:])
            for a in range(NSA):
                stt(jkB[:], ecol0[:], ef[:, a:a + 1], obs[:], ALU.is_equal, ALU.mult, accum=obs8[:, a:a + 1])
            ts('dve', isov[:], posf[:], 128.0, None, ALU.is_ge)
            ts('dve', qf[:], posf[:], -128.0, 0.0, ALU.add, ALU.max)
            cp('dve', qi[:], qf[:])
            tss(q2[:], qi[:], 127, ALU.bitwise_and)
            cp('dve', pov[:], q2[:])
            tss(q2[:], qi[:], 7, ALU.arith_shift_right)
            cp('dve', bov[:], q2[:])
            tt('dve', bov[:], bov[:], obs8[:], ALU.add)
            ts('dve', bov[:], bov[:], float(NE), None, ALU.add)
            tt('dve', pov[:], pov[:], posf[:], ALU.subtract); tt('dve', pov[:], pov[:], isov[:], ALU.mult); tt('dve', pov[:], pov[:], posf[:], ALU.add)
            tt('dve', bov[:], bov[:], ef[:], ALU.subtract); tt('dve', bov[:], bov[:], isov[:], ALU.mult); tt('dve', bov[:], bov[:], ef[:], ALU.add)
            stt(Rf[:], pov[:], float(NBLK), bov[:], ALU.mult, ALU.add)
            ts('dve', Rf[:], Rf[:], 1.0, None, ALU.add)
            cp('dve', IDX[:], Rf[:])
            iota(RIA[:, :, 0], [[T, 2], [1024, NCTL], [1, 8], [0, 8]], 0, 8)
            cp('dve', RIA[:].bitcast(F32)[:, :, 1], W8A[:])
            dump("IDX", IDX[:], [128, NSA], U32); dump("EBI", EBI[:], [128, 2], I32); dump("cum", cum[:], [128, NE])
            for a in range(NSA):
                P.op('pool', lambda e, a=a: e.indirect_dma_start(
                    out=RINFO, out_offset=bass.IndirectOffsetOnAxis(ap=IDX[:, a:a + 1], axis=0),
                    in_=RIA[:, a, :], in_offset=None), [RIA[:], IDX[:]], [(RINFO, a)], dma=True)
        P.barrier()

    if upto >= 6:
        with ExitStack() as st:
            idxall = sb(st, "idxall", [128, NBLK, 2], I32)
            wgf = [sb(st, f"wgf{i}", [128, 8, 256]) for i in range(2)]; wuf = [sb(st, f"wuf{i}", [128, 8, 256]) for i in range(2)]
            wdf = [sb(st, f"wdf{i}", [128, 2, D]) for i in range(2)]
            wgb = [sb(st, f"wgb{i}", [128, 8, 256], BF16) for i in range(2)]; wub = [sb(st, f"wub{i}", [128, 8, 256], BF16) for i in range(2)]
            wdb = [sb(st, f"wdb{i}", [128, 2, D], BF16) for i in range(2)]
            xg = [sb(st, f"xg{i}", [128, D], BF16) for i in range(2)]; xTe = [sb(st, f"xTe{i}", [128, 8, 128], BF16) for i in range(2)]
            sge = [sb(st, f"sge{i}", [128, 256]) for i in range(2)]; hide = [sb(st, f"hide{i}", [128, 256], BF16) for i in range(2)]
            ybe = [sb(st, f"ybe{i}", [128, D], BF16) for i in range(2)]
            pTe = [ps(st, f"pTe{i}", [128, 1024], BF16) for i in range(2)]; pgue = [ps(st, f"pgue{i}", [128, 512]) for i in range(2)]
            pye = [ps(st, f"pye{i}", [128, 512]) for i in range(4)]
            dma(idxall[:].rearrange("p q t -> p (q t)"), RINFO[1:NROW + 1, :].rearrange("(p q) t -> p (q t)", p=128))
            evc = {}
            NBRUN = NBLK if upto >= 7 or T >= 2048 else NBLK

            def wload(o, src, i):
                ov = o.rearrange("p a n -> p (a n)")
                if i < NE:
                    return dma(ov, src[i * 128:(i + 1) * 128, :])
                j = i - NE
                def f(e, ov=ov, src=src, j=j):
                    if 'bc' not in evc:
                        evc['bc'] = e.to_reg(NE * 128 - 1)
                    return e.indirect_dma_start(
                        out=ov, out_offset=None, in_=src, in_offset=bass.IndirectOffsetOnAxis(ap=EBW[:, j:j + 1], axis=0),
                        bounds_check=evc['bc'], oob_is_err=False)
                return P.op('pool', f, [EBW[:], src], [o], dma=True)

            def issue_loads(i):
                bi = i % 2
                wload(wgf[bi][:], exp_wg, i)
                wload(wuf[bi][:], exp_wu, i)
                wload(wdf[bi][:], exp_wd, i)
                P.op('pool', lambda e, i=i, bi=bi: e.indirect_dma_start(
                    out=xg[bi][:], out_offset=None, in_=HX2_S,
                    in_offset=bass.IndirectOffsetOnAxis(ap=idxall[:, i, 0:1].bitcast(U32), axis=0)), [idxall[:], HX2_S], [xg[bi][:]], dma=True)

            issue_loads(0)
            for i in range(NBRUN):
                bi = i % 2
                if i + 1 < NBRUN:
                    issue_loads(i + 1)
                cp('dve', wgb[bi][:], wgf[bi][:]); cp('act', wub[bi][:], wuf[bi][:])
                cp('dve', wdb[bi][:, 0], wdf[bi][:, 0]); cp('act', wdb[bi][:, 1], wdf[bi][:, 1])
                for kc in range(8):
                    tr(pTe[bi][:, kc * 128:(kc + 1) * 128], xg[bi][:, kc * 128:(kc + 1) * 128], ident_b[:])
                cp('act', xTe[bi][:].rearrange("p k c -> p (k c)"), pTe[bi][:])
                for oc in range(2):
                    for kc in range(8):
                        mm(pgue[bi][:, oc * 128:(oc + 1) * 128], wgb[bi][:, kc, oc * 128:(oc + 1) * 128], xTe[bi][:, kc, :], kc == 0, kc == 7)
                for oc in range(2):
                    for kc in range(8):
                        mm(pgue[bi][:, 256 + oc * 128:256 + (oc + 1) * 128], wub[bi][:, kc, oc * 128:(oc + 1) * 128], xTe[bi][:, kc, :], kc == 0, kc == 7)
                act(sge[bi][:], pgue[bi][:, 0:256], AF.Silu)
                tt('dve', hide[bi][:], sge[bi][:], pgue[bi][:, 256:512], ALU.mult)
                wcol = idxall[:, i, 1:2].bitcast(F32)
                for half in range(2):
                    py_ = pye[bi * 2 + half]
                    for oc in range(2):
                        mm(py_[:], hide[bi][:, oc * 128:(oc + 1) * 128], wdb[bi][:, oc, half * 512:(half + 1) * 512], oc == 0, oc == 1)
                    if half == 0:
                        ts('dve', ybe[bi][:, 0:512], py_[:], wcol, None, ALU.mult)
                    else:
                        act(ybe[bi][:, 512:1024], py_[:], AF.Copy, scale=wcol)
                dma(Y_S[1 + i:1 + i + NBLK * 127 + 1:NBLK, :], ybe[bi][:])
        P.barrier()

    if upto >= 6:
        with ExitStack() as st:
            G2b = [sb(st, f"G2b{i}", [128, D]) for i in range(2)]; fgb = sb(st, "fgb", [128, D])
            x1l = [sb(st, f"x1l{i}", [128, D]) for i in range(2)]
            gthA = [[sb(st, f"gth{q}_{i}", [128, D], BF16) for i in range(8)] for q in range(2)]
            accA = [sb(st, f"acc{q}", [128, D]) for q in range(2)]; acc2A = [sb(st, f"accb{q}", [128, D]) for q in range(2)]
            outtA = [sb(st, f"outt{q}", [128, D]) for q in range(2)]; junk6 = sb(st, "junk6", [128, D], BF16)
            ss6 = sb(st, "ss6", [128, 1]); rstd6 = sb(st, "rstd6", [128, 1]); tmpc6 = sb(st, "tmpc6", [128, 1])
            for b in range(2):
                dma(G2b[b][:], MODBC[3, b])
            dma(fgb[:], final_g.partition_broadcast(128).rearrange("p o d -> p (o d)"))
            for b in range(2):
                for sidx in range(NSET):
                    ct = sidx // 8; j = sidx % 8; gset = b * NSET + sidx
                    r0 = ct * 1024 + j
                    rsl = slice(r0, r0 + 8 * (CPT - 1) + 1, 8)
                    xl = x1l[sidx % 2]; gth = gthA[sidx % 2]; acc = accA[sidx % 2]; acc2 = acc2A[sidx % 2]; outt = outtA[sidx % 2]
                    if sidx == 0:
                        dma(xl[:], X1_S[b, rsl, :])
                    if sidx + 1 < NSET:
                        r1_ = ((sidx + 1) // 8) * 1024 + (sidx + 1) % 8
                        dma(x1l[(sidx + 1) % 2][:], X1_S[b, r1_:r1_ + 8 * (CPT - 1) + 1:8, :])
                    for k in range(8):
                        P.op('pool', lambda e, k=k, gset=gset, g_=gth[k]: e.indirect_dma_start(
                            out=g_[:], out_offset=None, in_=Y_S,
                            in_offset=bass.IndirectOffsetOnAxis(ap=IDX[:, gset * 8 + k:gset * 8 + k + 1], axis=0)),
                            [IDX[:], Y_S], [gth[k][:]], dma=True)
                    tt('dve', acc[:], gth[0][:], gth[1][:], ALU.add)
                    tt('pool', acc2[:], gth[2][:], gth[3][:], ALU.add)
                    tt('dve', acc[:], acc[:], gth[4][:], ALU.add)
                    tt('pool', acc2[:], acc2[:], gth[5][:], ALU.add)
                    tt('dve', acc[:], acc[:], gth[6][:], ALU.add)
                    tt('pool', acc2[:], acc2[:], gth[7][:], ALU.add)
                    tt('dve', acc[:], acc[:], acc2[:], ALU.add)
                    tt('dve', acc[:], acc[:], G2b[b][:], ALU.mult)
                    tt('dve', acc[:], acc[:], xl[:], ALU.add)
                    act(junk6[:], acc[:], AF.Square, accum=ss6[:])
                    rsqrt_col(rstd6[:], ss6[:], 1.0 / D, tmpc6[:])
                    stt(outt[:], acc[:], rstd6[:, 0:1], fgb[:], ALU.mult, ALU.mult)
                    dma(out[b, rsl, :], outt[:])
        P.barrier()

    P.final_wait_all()
    sems = ExitStack()
    esems = {e: sems.enter_context(nc.semaphore("sem_" + e)) for e in Prog.ENG}
    dsems = {'dma': [sems.enter_context(nc.semaphore(f"dsem{i}")) for i in range(NDSEM)],
             'sw': [sems.enter_context(nc.semaphore(f"ssem{i}")) for i in range(NDSEM)]}
    run = P.emit(esems, dsems)
    with nc.Block() as block:
        @block.tensor
        def _(e):
            run('pe')

        @block.vector
        def _(e):
            run('dve')

        @block.scalar
        def _(e):
            run('act')

        @block.gpsimd
        def _(e):
            run('pool')

        @block.sync
        def _(e):
            run('sp')
    sems.close()
    es.close()
    return nc, dbg_outs


INPUT_NAMES = ['x', 'c', 'ctx', 'c_ctx', 'ada_w', 'ada_b', 'norm1_g', 'norm2_g', 'w_in', 'gla_wa_f', 'gla_ba_f',
               'gla_wa_b', 'gla_ba_b', 'gla_norm_g', 's5_lam_re_f', 's5_lam_im_f', 's5_log_step_f', 's5_lam_re_b',
               's5_lam_im_b', 's5_log_step_b', 's5_b_re', 's5_b_im', 's5_c_re_f', 's5_c_im_f', 's5_c_re_b',
               's5_c_im_b', 's5_d', 's5_glu_w', 's5_glu_b', 'w_out', 'router_w', 'router_b', 'exp_w_gate',
               'exp_w_up', 'exp_w_down', 'sh_w_gate', 'sh_w_up', 'sh_w_down', 'final_norm_g']


def make_in_maps(inputs, ncores):
    f = lambda a: np.ascontiguousarray(np.asarray(a, dtype=np.float32))
    shared = {}
    for k in INPUT_NAMES:
        if k in ('x', 'c', 'ctx'):
            continue
        a = f(inputs[k])
        if k == 'c_ctx':
            a = a.reshape(1, D)
        elif k == 'final_norm_g':
            a = a.reshape(1, D)
        else:
            a = a[0]
            if a.ndim == 1:
                a = a.reshape(1, -1)
            if k.startswith('s5_c_'):
                a = a.reshape(512, 64)
            if k in ('exp_w_gate', 'exp_w_up'):
                a = a.reshape(NE, 8, 128, 256).transpose(0, 2, 1, 3).reshape(NE * 128, 2048)
            if k == 'exp_w_down':
                a = a.reshape(NE, 2, 128, 1024).transpose(0, 2, 1, 3).reshape(NE * 128, 2048)
        shared[k] = np.ascontiguousarray(a)
    xs = f(inputs['x']); cs = f(inputs['c']); cx = f(inputs['ctx'])
    maps = []
    for i in range(ncores):
        m = dict(shared)
        m['x'] = np.ascontiguousarray(xs[2 * i:2 * i + 2])
        m['c'] = np.ascontiguousarray(cs[2 * i:2 * i + 2])
        m['ctx'] = np.ascontiguousarray(cx[2 * i:2 * i + 2])
        maps.append(m)
    return maps


def kernel(**inputs):
    ncores = 8
    T = inputs['x'].shape[1]
    CT = inputs['ctx'].shape[1]
    nc, _ = build_program(T=T, CT=CT, NB=2)
    maps = make_in_maps(inputs, ncores)
    res = run_bass_kernel_spmd(nc, maps, core_ids=list(range(ncores)))
    outs = [np.asarray(r["out"], dtype=np.float32) for r in res.results]
    return np.concatenate(outs, axis=0)
```

```python
import numpy as np
import math
from contextlib import ExitStack
import concourse.bass as bass
import concourse.mybir as mybir
from concourse.bass_utils import run_bass_kernel_spmd

F32 = mybir.dt.float32
BF16 = mybir.dt.bfloat16
I32 = mybir.dt.int32
U32 = mybir.dt.uint32
ALU = mybir.AluOpType
AF = mybir.ActivationFunctionType
AX = mybir.AxisListType

D = 1024
DIN = 2080
NE = 256
EPS = 1e-6
NDSEM = 24
import os
FASTPE = tuple(os.environ.get('FASTPE', 'mm,tr').split(','))


class Prog:
    ENG = ['pe', 'dve', 'act', 'pool', 'sp']

    def __init__(self, nc):
        self.nc = nc
        self.ops = {e: [] for e in self.ENG}
        self.res = {}
        self.dmas = {'dma': [], 'sw': []}
        self.pending = {e: [] for e in self.ENG}
        self.last = {e: None for e in self.ENG}

    def _key(self, item):
        if isinstance(item, tuple):
            return (item[0].tensor.name, item[1])
        return (item.tensor.name, None)

    pemode = None

    def op(self, eng, fn, reads=(), writes=(), dma=False, pemode=None):
        deps = list(self.pending[eng])
        self.pending[eng] = []
        rk = [self._key(i) for i in reads]
        wk = [self._key(i) for i in writes]
        for k in rk:
            r = self.res.get(k)
            if r and r[0] is not None:
                deps.append(r[0])
        for k in wk:
            r = self.res.get(k)
            if r:
                if r[0] is not None:
                    deps.append(r[0])
                for en, ix in r[1].items():
                    deps.append((en, ix))
                deps.extend(r[2])
        o = {'fn': fn, 'dma': None, 'signal': False, 'eng': eng}
        if dma:
            kind = 'sw' if eng == 'pool' else 'dma'
            lst = self.dmas[kind]
            n = len(lst)
            tok = (kind, n)
            if n >= NDSEM:
                deps.append((kind, n - NDSEM))
            lst.append(o)
            o['dma'] = kind
            o['dn'] = n
        else:
            tok = (eng, len(self.ops[eng]))
        o['deps'] = set(deps)
        o['deps'].discard(tok)
        if eng == 'pe' and not dma:
            def slow_(m):
                return m is None or 'float32' in m[3] or m[1] < 128 or m[2] < 128 or (m[0] not in FASTPE)
            slow = slow_(pemode) or slow_(self.pemode)
            if not slow:
                o['deps'] = {d for d in o['deps'] if d[0] != 'pe'}
            if (slow or pemode != self.pemode) and self.last['pe'] is not None:
                o['deps'].add(self.last['pe'])
                if not slow:
                    o['drain'] = True
            self.pemode = pemode
        self.ops[eng].append(o)
        if not dma:
            self.last[eng] = tok
        for k in rk:
            r = self.res.setdefault(k, [None, {}, []])
            if dma:
                r[2].append(tok)
            else:
                r[1][eng] = tok[1]
        for k in wk:
            self.res[k] = [tok, {}, []]
        return tok

    def barrier(self):
        toks = [t for t in self.last.values() if t is not None]
        for kind, lst in self.dmas.items():
            toks += [(kind, i) for i in range(max(0, len(lst) - NDSEM), len(lst))]
        for e in self.ENG:
            self.pending[e] = list(toks)
        self.res = {}

    def emit(self, esems, dsems):
        nc = self.nc
        for e in self.ENG:
            for o in self.ops[e]:
                for d in o['deps']:
                    if d[0] not in ('dma', 'sw'):
                        self.ops[d[0]][d[1]]['signal'] = True
        val = {}
        for e in self.ENG:
            cnt = 0
            for i, o in enumerate(self.ops[e]):
                if o['dma']:
                    continue
                if o['signal']:
                    cnt += 1
                    val[(e, i)] = cnt
        engobj = {'pe': nc.tensor, 'dve': nc.vector, 'act': nc.scalar, 'pool': nc.gpsimd, 'sp': nc.sync}

        def tokval(d):
            if d[0] in ('dma', 'sw'):
                n = d[1]
                return (d[0], n % NDSEM), dsems[d[0]][n % NDSEM], 16 * (n // NDSEM + 1)
            return ('e', d[0]), esems[d[0]], val[d]

        def run(e):
            eo = engobj[e]
            seen = {}
            for i, o in enumerate(self.ops[e]):
                for d in sorted(o['deps'], key=str):
                    k, sem, v = tokval(d)
                    if seen.get(k, 0) >= v:
                        continue
                    seen[k] = v
                    eo.wait_ge(sem, v)
                if o.get('drain'):
                    eo.drain()
                ins = o['fn'](eo)
                if o['dma']:
                    n = o['dn']
                    ins.then_inc(dsems[o['dma']][n % NDSEM], 16)
                elif o['signal']:
                    ins.then_inc(esems[e], 1)
        return run

    def final_wait_all(self):
        self.barrier()
        self.op('sp', lambda e: e.nop(), [], [])


def build_program(T=2048, CT=256, NB=2, debug=None, upto=99):
    nc = bass.Bass("TRN2", target_bir_lowering=False)
    P = Prog(nc)
    NT = T // 128
    NCT = CT // 128
    NKX = T // 8
    NKC = CT // 8
    NK = NKX + NKC
    NCTL = max(1, T // 1024)
    CPT = min(128, NKX)
    NSET = NCTL * 8
    CAP = 128 * NB
    NROW = 128 * 2 * NE

    def din(name, shape, dt=F32):
        return nc.dram_tensor(name, list(shape), dt, kind="ExternalInput").ap()

    def dscr(name, shape, dt=F32):
        return nc.dram_tensor(name, list(shape), dt, kind="Internal").ap()

    x = din("x", [2, T, D]); cvec = din("c", [2, D]); ctx = din("ctx", [2, CT, D]); c_ctx = din("c_ctx", [1, D])
    ada_w = din("ada_w", [D, 6 * D]); ada_b = din("ada_b", [1, 6 * D])
    norm1_g = din("norm1_g", [1, D]); norm2_g = din("norm2_g", [1, D])
    w_in = din("w_in", [D, DIN])
    gla_wa = [din("gla_wa_f", [16, 256]), din("gla_wa_b", [16, 256])]
    gla_ba = [din("gla_ba_f", [1, 256]), din("gla_ba_b", [1, 256])]
    gla_norm_g = din("gla_norm_g", [1, 128])
    lam_re = [din("s5_lam_re_f", [32, 64]), din("s5_lam_re_b", [32, 64])]
    lam_im = [din("s5_lam_im_f", [32, 64]), din("s5_lam_im_b", [32, 64])]
    log_step = [din("s5_log_step_f", [1, 32]), din("s5_log_step_b", [1, 32])]
    s5_b = [din("s5_b_re", [32, 64, 16]), din("s5_b_im", [32, 64, 16])]
    s5_c = [[din("s5_c_re_f", [512, 64]), din("s5_c_im_f", [512, 64])],
            [din("s5_c_re_b", [512, 64]), din("s5_c_im_b", [512, 64])]]
    s5_d = din("s5_d", [1, 512]); s5_glu_w = din("s5_glu_w", [512, 512]); s5_glu_b = din("s5_glu_b", [1, 512])
    w_out = din("w_out", [D, D]); router_w = din("router_w", [D, NE]); router_b = din("router_b", [1, NE])
    exp_wg = din("exp_w_gate", [NE * 128, 2048]); exp_wu = din("exp_w_up", [NE * 128, 2048]); exp_wd = din("exp_w_down", [NE * 128, 2048])
    sh_wg = din("sh_w_gate", [D, 256]); sh_wu = din("sh_w_up", [D, 256]); sh_wd = din("sh_w_down", [256, D])
    final_g = din("final_norm_g", [1, D])
    out = nc.dram_tensor("out", [2, T, D], F32, kind="ExternalOutput").ap()

    WIN_BF = dscr("win_bf", [D, DIN], BF16); WOUT_BF = dscr("wout_bf", [D, D], BF16)
    GLU_BF = dscr("glu_bf", [512, 512], BF16); RW_BF = dscr("rw_bf", [D, NE], BF16)
    SHG_BF = dscr("shg_bf", [D, 256], BF16); SHU_BF = dscr("shu_bf", [D, 256], BF16); SHD_BF = dscr("shd_bf", [256, D], BF16)
    MODBC = dscr("modbc", [4, 2, 128, D])
    TOEP_S = dscr("toep_s", [128, 32 * 128], BF16)
    WS_S = dscr("ws_s", [128, 32 * 2 * 2 * 64], BF16)
    WO_S = dscr("wo_s", [128, 32 * 2 * 128], BF16)
    A8_S = dscr("a8_s", [128, 128])
    U_S = dscr("u_s", [2, T, 512], BF16); UC_S = dscr("uc_s", [2, CT, 512], BF16)
    X1_S = dscr("x1_s", [2, T, D]); HX2_S = dscr("hx2_s", [2 * T, D], BF16)
    RINFO = dscr("rinfo", [NROW + 1, 2], I32)
    Y_S = dscr("y_s", [NROW + 1, D], BF16)

    dbg_outs = {}

    def dump(name, ap, shape, dt=F32):
        if debug is None or name not in debug:
            return
        t = nc.dram_tensor("dbg_" + name, list(shape), dt, kind="ExternalOutput").ap()
        dbg_outs[name] = t
        P.op('sp', lambda e, o=t, i=ap: e.dma_start(out=o, in_=i), [ap], [t], dma=True)

    es = ExitStack()

    uid = [0]

    def sb(st, name, shape, dt=F32):
        uid[0] += 1
        return st.enter_context(nc.sbuf_tensor(f"{name}_{uid[0]}", list(shape), dt))

    def ps(st, name, shape, dt=F32):
        uid[0] += 1
        return st.enter_context(nc.psum_tensor(f"{name}_{uid[0]}", list(shape), dt))

    def dma(o, i, q='sp', **kw):
        return P.op(q, lambda e, o=o, i=i, kw=kw: e.dma_start(out=o, in_=i, **kw), [i], [o], dma=True)

    def rnd_(n):
        return 32 if n <= 32 else (64 if n <= 64 else 128)

    def mm(o, l, r, st=True, sp=True, extra_r=()):
        mode = ('mm', rnd_(l.shape[0]), rnd_(int(np.prod(l.shape[1:]))), str(l.dtype))
        return P.op('pe', lambda e, o=o, l=l, r=r, st=st, sp=sp: e.matmul(o, l, r, start=st, stop=sp),
                    [l, r] + list(extra_r), [o], pemode=mode)

    def tr(o, i, ident):
        mode = ('tr', rnd_(i.shape[0]), rnd_(int(np.prod(i.shape[1:]))), str(i.dtype))
        return P.op('pe', lambda e, o=o, i=i, idn=ident: e.transpose(o, i, idn), [i, ident], [o], pemode=mode)

    def U_(a):
        return a[0] if isinstance(a, tuple) else a

    def tt(eng, o, a, b, op):
        return P.op(eng, lambda e, o=U_(o), a=U_(a), b=U_(b), op=op: e.tensor_tensor(out=o, in0=a, in1=b, op=op), [a, b], [o])

    def ts(eng, o, a, s1, s2, op0, op1=None, accum=None):
        rd = [a] + [s for s in (s1, s2) if isinstance(s, bass.AP)]
        wr = [o] + ([accum] if accum is not None else [])

        def f(e, o=o, a=a, s1=s1, s2=s2, op0=op0, op1=op1, accum=accum):
            kw = {}
            if accum is not None:
                kw['accum_out'] = accum
            if op1 is None:
                return e.tensor_scalar(out=o, in0=a, scalar1=s1, scalar2=None, op0=op0, **kw)
            return e.tensor_scalar(out=o, in0=a, scalar1=s1, scalar2=s2, op0=op0, op1=op1, **kw)
        return P.op(eng, f, rd, wr)

    def stt(o, a, s, b, op0, op1, accum=None):
        rd = [a, b] + ([s] if isinstance(s, bass.AP) else [])
        wr = [o] + ([accum] if accum is not None else [])

        def f(e, o=o, a=a, s=s, b=b, op0=op0, op1=op1, accum=accum):
            kw = {}
            if accum is not None:
                kw['accum_out'] = accum
            return e.scalar_tensor_tensor(out=o, in0=a, scalar=s, in1=b, op0=op0, op1=op1, **kw)
        return P.op('dve', f, rd, wr)

    def act(o, i, func, bias=None, scale=None, accum=None):
        rd = [i] + [s for s in (bias, scale) if isinstance(s, bass.AP)]
        wr = [o] + ([accum] if accum is not None else [])

        def f(e, o=o, i=i, func=func, bias=bias, scale=scale, accum=accum):
            kw = {}
            if bias is not None:
                kw['bias'] = bias
            if scale is not None:
                kw['scale'] = scale
            if accum is not None:
                kw['accum_out'] = accum
            return e.activation(out=o, in_=i, func=func, **kw)
        return P.op('act', f, rd, wr)

    def cp(eng, o, i):
        if eng == 'act':
            return P.op('act', lambda e, o=U_(o), i=U_(i): e.copy(out=o, in_=i), [i], [o])
        return P.op(eng, lambda e, o=U_(o), i=U_(i): e.tensor_copy(out=o, in_=i), [i], [o])

    def memset(eng, o, v):
        return P.op(eng, lambda e, o=o, v=v: e.memset(o, v), [], [o])

    def recip(o, i):
        return P.op('dve', lambda e, o=o, i=i: e.reciprocal(out=o, in_=i), [i], [o])

    def iota(o, pattern, base, cm):
        return P.op('pool', lambda e, o=o: e.iota(o, pattern=pattern, base=base, channel_multiplier=cm), [], [o])

    def rsqrt_col(o, i, scale, tmp):
        ts('dve', tmp, i, scale, EPS, ALU.mult, ALU.add)
        act(tmp, tmp, AF.Sqrt)
        recip(o, tmp)

    st0 = es
    ident_f = sb(st0, "ident_f", [128, 128]); ident_b = sb(st0, "ident_b", [128, 128], BF16)
    maskF = sb(st0, "maskF", [128, 128]); maskB = sb(st0, "maskB", [128, 128])
    SLm = sb(st0, "SLm", [128, 128]); SUm = sb(st0, "SUm", [128, 128])
    SU_b = sb(st0, "SU_b", [128, 128], BF16); ones_b = sb(st0, "ones_b", [128, 128], BF16)
    ones_f = sb(st0, "ones_f", [128, 128])
    iot = sb(st0, "iot", [128, 128], I32); iotf = sb(st0, "iotf", [128, 128])
    AB = sb(st0, "AB", [128, 4, 8, 3])
    iota(iot[:], [[1, 128]], 0, -1)
    cp('dve', iotf[:], iot[:])
    ts('dve', ident_f[:], iotf[:], 0.0, None, ALU.is_equal)
    cp('dve', ident_b[:], ident_f[:])
    ts('dve', maskF[:], iotf[:], 0.0, None, ALU.is_ge)
    ts('dve', maskB[:], iotf[:], 0.0, None, ALU.is_le)
    ts('dve', SLm[:], iotf[:], 0.0, None, ALU.is_lt)
    ts('dve', SUm[:], iotf[:], 0.0, None, ALU.is_gt)
    cp('dve', SU_b[:], SUm[:])
    memset('dve', ones_f[:], 1.0)
    memset('dve', ones_b[:], 1.0)

    with ExitStack() as st:
        stg = [sb(st, f"wstg{i}", [128, DIN]) for i in range(2)]
        stb = [sb(st, f"wstb{i}", [128, DIN], BF16) for i in range(2)]
        cnt = 0
        for (src, dst, K, N) in [(w_in, WIN_BF, D, DIN), (w_out, WOUT_BF, D, D), (s5_glu_w, GLU_BF, 512, 512),
                                 (router_w, RW_BF, D, NE), (sh_wg, SHG_BF, D, 256), (sh_wu, SHU_BF, D, 256),
                                 (sh_wd, SHD_BF, 256, D)]:
            for kc in range(K // 128):
                a = stg[cnt % 2]; b = stb[cnt % 2]
                dma(a[:, 0:N], src[kc * 128:(kc + 1) * 128, :])
                cp(['dve', 'act', 'pool'][cnt % 3], b[:, 0:N], a[:, 0:N])
                dma(dst[kc * 128:(kc + 1) * 128, :], b[:, 0:N])
                cnt += 1
    P.barrier()

    with ExitStack() as st:
        cT = sb(st, "cT", [128, 8, 3]); sT = sb(st, "sT", [128, 8, 3])
        sBC = sb(st, "sBC", [128, 2, 8, 128])
        abT = sb(st, "abT", [128, 48]); n1g = sb(st, "n1g", [128, 8]); n2g = sb(st, "n2g", [128, 8])
        modsb = sb(st, "modsb", [128, 48, 3])
        aw = [sb(st, f"aw{i}", [128, 8, 512]) for i in range(2)]
        abrow = [sb(st, f"abrow{i}", [1, 512]) for i in range(2)]
        bct = [sb(st, f"bct{i}", [128, 512]) for i in range(2)]
        n2bc = sb(st, "n2bc", [128, D])
        modps = ps(st, "modps", [128, 192])
        bcps = [ps(st, f"bcps{i}", [128, 512]) for i in range(2)]
        NCK = dict(allow_slow_non_contiguous=True)
        cT2 = sb(st, "cT2", [128, 3, 8])
        for j in range(2):
            dma(cT2[:, j, :], cvec[j:j + 1, :].rearrange("o (kc p) -> p (o kc)", p=128), **NCK)
        dma(cT2[:, 2, :], c_ctx.rearrange("o (kc p) -> p (o kc)", p=128), **NCK)
        cp('dve', cT[:], cT2[:].rearrange("p j k -> p k j"))
        dma(abT[:], ada_b.rearrange("o (fc p) -> p (o fc)", p=128), **NCK)
        dma(n1g[:], norm1_g.rearrange("o (kc p) -> p (o kc)", p=128), **NCK)
        dma(n2g[:], norm2_g.rearrange("o (kc p) -> p (o kc)", p=128), **NCK)
        dma(n2bc[:], norm2_g.partition_broadcast(128).rearrange("p o d -> p (o d)"))
        act(sT[:], cT[:], AF.Silu)
        for j in range(2):
            for kc in range(8):
                cp('dve', sBC[:, j, kc, :], sT[:, kc, j:j + 1].broadcast_to([128, 128]))
        bci = 0
        for nb in range(12):
            a = aw[nb % 2]
            dma(a[:], ada_w[:, nb * 512:(nb + 1) * 512].rearrange("(kc p) n -> p kc n", p=128))
            mi = nb // 2; half = nb % 2
            for f4 in range(4):
                fc = nb * 4 + f4
                for kc in range(8):
                    mm(modps[:, fc * 4:fc * 4 + 3], a[:, kc, f4 * 128:(f4 + 1) * 128], sT[:, kc, :], kc == 0, kc == 7)
            if mi in (2, 3, 4, 5):
                ar = abrow[nb % 2]
                dma(ar[:], ada_b[:, nb * 512:(nb + 1) * 512])
                for j in range(2):
                    pb = bcps[bci % 2]; bt = bct[bci % 2]; bci += 1
                    for kc in range(8):
                        mm(pb[:], sBC[:, j, kc, :], a[:, kc, :], kc == 0, False)
                    mm(pb[:], ones_f[0:1, :], ar[:], False, True)
                    slot = {2: 0, 4: 1, 3: 2, 5: 3}[mi]
                    if mi == 4:
                        stt(bt[:], pb[:], 1.0, n2bc[:, half * 512:(half + 1) * 512], ALU.add, ALU.mult)
                    else:
                        cp('act', bt[:], pb[:])
                    dma(MODBC[slot, j, :, half * 512:(half + 1) * 512], bt[:])
        tt('dve', modsb[:], modps[:].rearrange("p (f q) -> p f q", q=4)[:, :, 0:3],
           abT[:].unsqueeze(2).broadcast_to([128, 48, 3]), ALU.add)
        stt(AB[:, 0, :, :], modsb[:, 8:16, :], 1.0, n1g[:].unsqueeze(2).broadcast_to([128, 8, 3]), ALU.add, ALU.mult)
        cp('dve', AB[:, 1, :, :], modsb[:, 0:8, :])
        stt(AB[:, 2, :, :], modsb[:, 32:40, :], 1.0, n2g[:].unsqueeze(2).broadcast_to([128, 8, 3]), ALU.add, ALU.mult)
        cp('dve', AB[:, 3, :, :], modsb[:, 24:32, :])
        dump("AB", AB[:], [128, 4, 8, 3])
    P.barrier()
    dump("modbc", MODBC[:, :, 0:1, :], [4, 2, 1, D])


    NCK = dict(allow_slow_non_contiguous=True)
    PI = math.pi
    if upto >= 2:
      with ExitStack() as st:
        lamT = sb(st, "lamT", [128, 2, 2, 32]); lst = sb(st, "lst", [128, 2, 32])
        bT = sb(st, "bT", [128, 2, 32, 16]); cT5 = sb(st, "cT5", [128, 2, 2, 512]); cn = sb(st, "cn", [128, 4, 128])
        dtt = sb(st, "dtt", [128, 2, 32]); lrd = sb(st, "lrd", [128, 2, 32]); th = sb(st, "th", [128, 2, 32])
        evi = sb(st, "evi", [128, 16], I32); evec = sb(st, "evec", [128, 16])
        PW = sb(st, "PW", [128, 2, 2, 32, 16])
        sm = [sb(st, f"sm{i}", [128, 2, 32]) for i in range(8)]
        fre = sb(st, "fre", [128, 2, 32]); fim = sb(st, "fim", [128, 2, 32])
        Bb = sb(st, "Bb", [128, 2, 2, 32, 16]); w1 = sb(st, "w1", [128, 2, 32, 16]); w2 = sb(st, "w2", [128, 2, 32, 16])
        A8 = sb(st, "A8", [128, 2, 2, 32])
        colTi = sb(st, "colTi", [128, 128], I32); colTf = sb(st, "colTf", [128, 128])
        rowSi = sb(st, "rowSi", [128, 1], I32); rowSf = sb(st, "rowSf", [128, 1])
        mTf = sb(st, "mTf", [128, 128]); mTb = sb(st, "mTb", [128, 128]); Dcol = sb(st, "Dcol", [128, 32])
        pct = ps(st, "pct", [128, 512]); ptp = [ps(st, f"ptp{i}", [128, 512]) for i in range(2)]
        pws = [ps(st, f"pws{i}", [128, 512]) for i in range(2)]
        st2 = ExitStack()
        arg2 = sb(st2, "arg2", [128, 2, 1024]); magl = sb(st2, "magl", [128, 1024]); mag = sb(st2, "mag", [128, 1024])
        nfi = sb(st2, "nfi", [128, 2, 1024], I32); nf = sb(st2, "nf", [128, 2, 1024]); rr = sb(st2, "rr", [128, 2, 1024])
        mk = sb(st2, "mk", [128, 2, 1024]); scs = sb(st2, "scs", [128, 2, 1024])
        for hf in range(2):
            hs = slice(hf * 64, hf * 64 + 64)
            for d_ in range(2):
                dma(lamT[hs, d_, 0, :], lam_re[d_].rearrange("g p -> p g"), **NCK)
                dma(lamT[hs, d_, 1, :], lam_im[d_].rearrange("g p -> p g"), **NCK)
            for ri in range(2):
                dma(bT[hs, ri, :, :], s5_b[ri].rearrange("g p h -> p g h"))
        for d_ in range(2):
            dma(lst[:, d_, :], log_step[d_].partition_broadcast(128).rearrange("p o g -> p (o g)"))
        for s_ in range(8):
            dma(Dcol[s_ * 16:(s_ + 1) * 16, :], s5_d.rearrange("o (g h) -> h (o g)", h=16), **NCK)
        for d_ in range(2):
            for ri in range(2):
                for hf in range(2):
                    dma(cn[:, :, hf * 64:(hf + 1) * 64], s5_c[d_][ri].rearrange("(rt r) p -> r rt p", r=128))
                for rt in range(4):
                    tr(pct[:, rt * 128:(rt + 1) * 128], cn[:, rt, :], ident_f[:])
                cp('dve', cT5[:, d_, ri, :], pct[:])
        iota(colTi[:].rearrange("p (t h) -> p t h", h=16), [[1, 8], [0, 16]], 0, 0)
        cp('dve', colTf[:], colTi[:])
        iota(rowSi[:], [[0, 1]], 0, 1)
        P.op('dve', lambda e: e.tensor_single_scalar(out=rowSi[:], in_=rowSi[:], scalar=4, op=ALU.arith_shift_right), [rowSi[:]], [rowSi[:]])
        cp('dve', rowSf[:], rowSi[:])
        ts('dve', mTf[:], colTf[:], rowSf[:, 0:1], None, ALU.is_ge)
        ts('dve', mTb[:], colTf[:], rowSf[:, 0:1], None, ALU.is_le)
        act(dtt[:], lst[:], AF.Exp)
        tt('dve', lrd[:], lamT[:, :, 0, :], dtt[:], ALU.mult)
        tt('dve', th[:], lamT[:, :, 1, :], dtt[:], ALU.mult)
        iota(evi[:], [[1, 16]], -7, 0)
        cp('dve', evec[:], evi[:])
        ev_b = evec[:].unsqueeze(1).broadcast_to([128, 64, 16])
        tt('dve', arg2[:, 0, :].rearrange("p (a e) -> p a e", e=16),
           th[:].rearrange("p d g -> p (d g)").unsqueeze(2).broadcast_to([128, 64, 16]), ev_b, ALU.mult)
        tt('dve', magl[:].rearrange("p (a e) -> p a e", e=16),
           lrd[:].rearrange("p d g -> p (d g)").unsqueeze(2).broadcast_to([128, 64, 16]), ev_b, ALU.mult)
        act(mag[:], magl[:], AF.Exp)
        ts('dve', arg2[:, 1, :], arg2[:, 0, :], PI / 2, None, ALU.add)
        ts('dve', nf[:], arg2[:], 1.0 / (2 * PI), None, ALU.mult)
        cp('dve', nfi[:], nf[:])
        cp('dve', nf[:], nfi[:])
        stt(rr[:], nf[:], -2 * PI, arg2[:], ALU.mult, ALU.add)
        ts('dve', mk[:], rr[:], PI, None, ALU.is_gt)
        stt(rr[:], mk[:], -2 * PI, rr[:], ALU.mult, ALU.add)
        ts('dve', mk[:], rr[:], -PI, None, ALU.is_lt)
        stt(rr[:], mk[:], 2 * PI, rr[:], ALU.mult, ALU.add)
        ts('dve', rr[:], rr[:], -3.141592, 3.141592, ALU.max, ALU.min)
        act(scs[:], rr[:], AF.Sin)
        tt('dve', PW[:, 0].rearrange("p d g e -> p (d g e)"), mag[:], scs[:, 1, :], ALU.mult)
        tt('dve', PW[:, 1].rearrange("p d g e -> p (d g e)"), mag[:], scs[:, 0, :], ALU.mult)
        P.barrier()
        st2.close()
        Lr = sb(st, "Lr", [128, 32, 8, 16]); Li = sb(st, "Li", [128, 32, 8, 16])
        Rr = sb(st, "Rr", [128, 32, 8, 16]); Ri = sb(st, "Ri", [128, 32, 8, 16])
        q1 = sb(st, "q1", [128, 32, 8, 16]); q2 = sb(st, "q2", [128, 32, 8, 16])
        T32 = sb(st, "T32", [128, 32, 128]); TOb = sb(st, "TOb", [128, 32, 128], BF16); tmpT = sb(st, "tmpT", [128, 4, 128])
        WS = sb(st, "WS", [128, 32, 2, 2, 64], BF16); WO = sb(st, "WO", [128, 32, 2, 128], BF16)
        are = PW[:, 0, :, :, 8]; aim = PW[:, 1, :, :, 8]
        lre = lamT[:, :, 0, :]; lim = lamT[:, :, 1, :]
        tt('dve', sm[0][:], lre, lre, ALU.mult)
        tt('dve', sm[1][:], lim, lim, ALU.mult)
        tt('dve', sm[0][:], sm[0][:], sm[1][:], ALU.add)
        recip(sm[1][:], sm[0][:])
        ts('dve', sm[2][:], are, -1.0, None, ALU.add)
        tt('dve', sm[3][:], sm[2][:], lre, ALU.mult)
        tt('dve', sm[4][:], aim, lim, ALU.mult)
        tt('dve', sm[3][:], sm[3][:], sm[4][:], ALU.add)
        tt('dve', fre[:], sm[3][:], sm[1][:], ALU.mult)
        tt('dve', sm[5][:], aim, lre, ALU.mult)
        tt('dve', sm[6][:], sm[2][:], lim, ALU.mult)
        tt('dve', sm[5][:], sm[5][:], sm[6][:], ALU.subtract)
        tt('dve', fim[:], sm[5][:], sm[1][:], ALU.mult)
        fre_b = fre[:].unsqueeze(3).broadcast_to([128, 2, 32, 16]); fim_b = fim[:].unsqueeze(3).broadcast_to([128, 2, 32, 16])
        bre_b = bT[:, 0].unsqueeze(1).broadcast_to([128, 2, 32, 16]); bim_b = bT[:, 1].unsqueeze(1).broadcast_to([128, 2, 32, 16])
        tt('dve', w1[:], fre_b, bre_b, ALU.mult); tt('dve', w2[:], fim_b, bim_b, ALU.mult)
        tt('dve', Bb[:, 0], w1[:], w2[:], ALU.subtract)
        tt('dve', w1[:], fre_b, bim_b, ALU.mult); tt('dve', w2[:], fim_b, bre_b, ALU.mult)
        tt('dve', Bb[:, 1], w1[:], w2[:], ALU.add)

        def esl(e0, step):
            i0 = e0 + 7
            if step == 1:
                return slice(i0, i0 + 8)
            stop = i0 - 8
            return slice(i0, stop if stop >= 0 else None, -1)

        def cprod(dre, dim_, Xre, Xim, d_, e0, step, neg_im):
            sl = esl(e0, step)
            Pre = PW[:, 0, d_, :, sl].unsqueeze(3).broadcast_to([128, 32, 8, 16])
            Pim = PW[:, 1, d_, :, sl].unsqueeze(3).broadcast_to([128, 32, 8, 16])
            Xr = Xre.unsqueeze(2).broadcast_to([128, 32, 8, 16]); Xi = Xim.unsqueeze(2).broadcast_to([128, 32, 8, 16])
            tt('dve', q1[:], Xr, Pre, ALU.mult); tt('pool', q2[:], Xi, Pim, ALU.mult)
            tt('dve', dre[:], q1[:], q2[:], ALU.subtract)
            tt('dve', q1[:], Xr, Pim, ALU.mult); tt('pool', q2[:], Xi, Pre, ALU.mult)
            if neg_im:
                stt(dim_[:], q1[:], -1.0, q2[:], ALU.mult, ALU.subtract)
            else:
                tt('dve', dim_[:], q1[:], q2[:], ALU.add)

        def c_of(d_, ri):
            return cT5[:, d_, ri, :].rearrange("p (g h) -> p g h", h=16)

        for d_ in range(2):
            Bre = Bb[:, 0, d_]; Bim = Bb[:, 1, d_]
            if d_ == 0:
                cprod(Lr, Li, Bre, Bim, 0, 0, -1, False); cprod(Rr, Ri, c_of(0, 0), c_of(0, 1), 0, 0, 1, True)
            else:
                cprod(Lr, Li, Bre, Bim, 1, 0, 1, False); cprod(Rr, Ri, c_of(1, 0), c_of(1, 1), 1, 0, -1, True)
            for gb in range(8):
                pt = ptp[gb % 2]
                for gi in range(4):
                    g = gb * 4 + gi
                    mm(pt[:, gi * 128:(gi + 1) * 128], Lr[0:64, g].rearrange("p s h -> p (s h)"),
                       Rr[0:64, g].rearrange("p s h -> p (s h)"), True, False)
                    mm(pt[:, gi * 128:(gi + 1) * 128], Li[0:64, g].rearrange("p s h -> p (s h)"),
                       Ri[0:64, g].rearrange("p s h -> p (s h)"), False, True)
                ptv = pt[:].rearrange("p (a c) -> p a c", a=4)
                if d_ == 0:
                    tt('dve', tmpT[:], ptv, mTf[:].unsqueeze(1).broadcast_to([128, 4, 128]), ALU.mult)
                    for gi in range(4):
                        g = gb * 4 + gi
                        stt(T32[:, g, :], ident_f[:], Dcol[:, g:g + 1], tmpT[:, gi, :], ALU.mult, ALU.add)
                else:
                    tt('dve', tmpT[:], ptv, mTb[:].unsqueeze(1).broadcast_to([128, 4, 128]), ALU.mult)
                    tt('dve', TOb[:, gb * 4:gb * 4 + 4, :], tmpT[:], T32[:, gb * 4:gb * 4 + 4, :], ALU.add)
            if d_ == 0:
                cprod(Lr, Li, Bre, Bim, 0, 7, -1, False)
            for ri, Lx in enumerate((Lr, Li)):
                for gb in range(4):
                    pw = pws[gb % 2]
                    for gi in range(8):
                        g = gb * 8 + gi
                        tr(pw[:, gi * 64:(gi + 1) * 64], Lx[0:64, g].rearrange("p s h -> p (s h)"), ident_f[0:64, 0:64])
                    cp('act', WS[:, gb * 8:gb * 8 + 8, d_, ri, :], pw[:].rearrange("p (a c) -> p a c", a=8))
            if d_ == 0:
                cprod(Rr, Ri, c_of(0, 0), c_of(0, 1), 0, 1, 1, True)
            else:
                cprod(Rr, Ri, c_of(1, 0), c_of(1, 1), 1, 8, -1, True)
            hs = slice(d_ * 64, d_ * 64 + 64)
            cp('act', WO[hs, :, 0, :], Rr[hs].rearrange("p g s h -> p g (s h)"))
            cp('act', WO[hs, :, 1, :], Ri[hs].rearrange("p g s h -> p g (s h)"))
            cp('dve', A8[hs, 0, 0, :], PW[hs, 0, d_, :, 15]); cp('dve', A8[hs, 0, 1, :], PW[hs, 0, d_, :, 15])
            ts('dve', A8[hs, 1, 0, :], PW[hs, 1, d_, :, 15], -1.0, None, ALU.mult)
            cp('dve', A8[hs, 1, 1, :], PW[hs, 1, d_, :, 15])
        dma(TOEP_S, TOb[:].rearrange("p g c -> p (g c)"))
        dma(WS_S, WS[:].rearrange("p g d r c -> p (g d r c)"))
        dma(WO_S, WO[:].rearrange("p g r c -> p (g r c)"))
        dma(A8_S, A8[:].rearrange("p a r g -> p (a r g)"))
        dump("TOb", TOb[:], [128, 32, 128], BF16)
        dump("PW", PW[:], [128, 2, 2, 32, 16])
        dump("Bb", Bb[:], [128, 2, 2, 32, 16])
      P.barrier()

    def reduce_sum(o, i):
        return P.op('dve', lambda e, o=o, i=i: e.reduce_sum(out=o, in_=i, axis=AX.X), [i], [o])

    NBLK = 2 * NE
    cum = sb(es, "cum", [128, NE]); IDX = sb(es, "IDX", [128, 2 * NSET * 8], U32)
    ecol = sb(es, "ecol", [128, NE]); ecoli = sb(es, "ecoli", [128, NE], I32); ecol0 = sb(es, "ecol0", [128, NE])
    V8A = sb(es, "V8A", [128, 2 * NSET * 8]); W8A = sb(es, "W8A", [128, 2 * NSET * 8])
    RIA = sb(es, "RIA", [128, 2 * NSET * 8, 2], I32); EBI = sb(es, "EBI", [128, 2], I32); EBW = sb(es, "EBW", [128, NE], U32)
    if upto >= 4:
        with ExitStack() as st:
            zt = sb(st, "zt", [128, NBLK * 2], I32); ztb = sb(st, "ztb", [1, D], BF16)
            memset('dve', zt[:], 0)
            memset('dve', ztb[:], 0.0)
            memset('dve', cum[:], 0.0)
            iota(ecoli[:], [[1, NE]], 1, 0)
            cp('dve', ecol[:], ecoli[:])
            ts('dve', ecol0[:], ecol[:], -1.0, None, ALU.add)
            dma(RINFO[1:NROW + 1, :].rearrange("(p q) t -> p (q t)", p=128), zt[:])
            dma(RINFO[0:1, :], zt[0:1, 0:2])
            dma(Y_S[0:1, :], ztb[:])
        P.barrier()

    for b in range(2):
        if upto < 1:
            break
        seqst = ExitStack()
        mixg = sb(seqst, f"mixg{b}", [128, 4, T], BF16)
        with ExitStack() as st:
            Wi = sb(st, "Wi", [128, 8, DIN], BF16)
            wa = sb(st, "wa", [16, 2, 256]); ba = sb(st, "ba", [1, 2, 256]); gng = sb(st, "gng", [128, 128])
            OF = sb(st, "OF", [128, NT, 512], BF16)
            xts = [sb(st, f"xt{i}", [128, D]) for i in range(2)]
            junk = sb(st, "junk", [128, D], BF16); xn = sb(st, "xn", [128, D], BF16)
            ss = sb(st, "ss", [128, 1]); rstd = sb(st, "rstd", [128, 1]); tmpc = sb(st, "tmpc", [128, 1])
            hT = sb(st, "hT", [128, 8, 128], BF16)
            lrT = sb(st, "lrT", [16, 128]); e1 = sb(st, "e1", [128, 256]); lsp = sb(st, "lsp", [128, 256])
            kdec = sb(st, "kdec", [128, 256]); kS = sb(st, "kS", [128, 256], BF16)
            Ep = sb(st, "Ep", [128, 256]); Em = sb(st, "Em", [128, 256])
            qc = sb(st, "qc", [128, 256], BF16); kI = sb(st, "kI", [128, 256], BF16)
            vb = sb(st, "vb", [128, 512], BF16); ub = sb(st, "ub", [128, 512], BF16)
            sTm = sb(st, "sTm", [128, 512], BF16)
            S32 = sb(st, "S32", [128, 2, 128]); Sbf = sb(st, "Sbf", [128, 2, 128], BF16)
            ot = sb(st, "ot", [128, 512]); osq = sb(st, "osq", [128, 512]); on = sb(st, "on", [128, 512])
            sg = sb(st, "sg", [128, 512]); go = sb(st, "go", [128, 512], BF16)
            ssh = sb(st, "ssh", [128, 4]); rsh = sb(st, "rsh", [128, 4]); tmp4 = sb(st, "tmp4", [128, 4])
            pT = ps(st, "pT", [128, 1024], BF16); pq = ps(st, "pq", [128, 512]); pkz = ps(st, "pkz", [128, 512])
            pk = pkz[:, 0:256]; pz = pkz[:, 256:512]; pv = ps(st, "pv", [128, 512]); pgu = ps(st, "pgu", [128, 512])
            pDc = ps(st, "pDc", [128, 512]); pD = pDc[:, 0:256]; pc = pDc[:, 256:512]; pss = ps(st, "pss", [128, 512])
            po = ps(st, "po", [128, 512])
            dma(Wi[:], WIN_BF.rearrange("(kc p) n -> p kc n", p=128))
            for d_ in range(2):
                dma(wa[:, d_, :], gla_wa[d_])
                dma(ba[:, d_, :], gla_ba[d_])
            dma(gng[:], gla_norm_g.partition_broadcast(128).rearrange("p o d -> p (o d)"))
            tcount = [0]

            def gla_tile(src, usrc, n, dirn, want_out, first_pass):
                jm = b if want_out else 2
                xt = xts[tcount[0] % 2]; tcount[0] += 1
                dma(xt[:], src[b, n * 128:(n + 1) * 128, :])
                act(junk[:], xt[:], AF.Square, accum=ss[:])
                rsqrt_col(rstd[:], ss[:], 1.0 / D, tmpc[:])
                ts('dve', xn[:], xt[:], rstd[:, 0:1], None, ALU.mult)
                for kc in range(8):
                    tr(pT[:, kc * 128:(kc + 1) * 128], xn[:, kc * 128:(kc + 1) * 128], ident_b[:])
                for kc in range(8):
                    if kc % 2 == 0:
                        ts('dve', hT[:, kc, :], pT[:, kc * 128:(kc + 1) * 128], AB[:, 0, kc, jm:jm + 1],
                           AB[:, 1, kc, jm:jm + 1], ALU.mult, ALU.add)
                    else:
                        act(hT[:, kc, :], pT[:, kc * 128:(kc + 1) * 128], AF.Identity,
                            bias=AB[:, 1, kc, jm:jm + 1], scale=AB[:, 0, kc, jm:jm + 1])
                for gi in range(4):
                    for kc in range(8):
                        mm(pq[:, gi * 128:(gi + 1) * 128], Wi[:, kc, gi * 128:(gi + 1) * 128], hT[:, kc, :], kc == 0, kc == 7)
                for kc in range(8):
                    mm(pk[:], hT[:, kc, :], Wi[:, kc, 256:512], kc == 0, kc == 7)
                for kc in range(8):
                    mm(pv[:], hT[:, kc, :], Wi[:, kc, 512:1024], kc == 0, kc == 7)
                c0 = 1536 + 16 * dirn
                for kc in range(8):
                    mm(pc[:, 0:128], Wi[:, kc, c0:c0 + 128], hT[:, kc, :], kc == 0, kc == 7)
                cp('act', lrT[:], pc[0:16, 0:128])
                if first_pass:
                    for kc in range(8):
                        mm(pgu[:], hT[:, kc, :], Wi[:, kc, 1568:2080], kc == 0, kc == 7)
                    cp('act', ub[:], pgu[:])
                    dma(usrc[b, n * 128:(n + 1) * 128, :], ub[:])
                elif want_out:
                    for kc in range(8):
                        mm(pgu[:], hT[:, kc, :], Wi[:, kc, 1024:1536], kc == 0, kc == 7)
                mm(pz[:], lrT[:], wa[:, dirn, :], True, False)
                mm(pz[:], ones_f[0:1, :], ba[:, dirn, :], False, True)
                act(e1[:], pz[:], AF.Exp, scale=-1.0)
                act(lsp[:], e1[:], AF.Ln, bias=1.0)
                mm(pD[:], (SLm if dirn == 0 else SUm)[:], lsp[:])
                act(kdec[:], pD[:], AF.Exp, scale=-1.0 / 16)
                tt('dve', kS[:], pk[:], kdec[:], ALU.mult)
                msk = maskF if dirn == 0 else maskB
                for hp in range(2):
                    mm(pc[:, hp * 128:(hp + 1) * 128], lsp[:, hp * 128:(hp + 1) * 128], msk[:])
                act(Ep[:], pc[:], AF.Exp, scale=-1.0 / 16)
                act(Em[:], pc[:], AF.Exp, scale=1.0 / 16)
                stt(qc[:], pq[:, 0:256], 0.125, Ep[:], ALU.mult, ALU.mult)
                tt('dve', kI[:], pq[:, 256:512], Em[:], ALU.mult)
                cp('act', vb[:], pv[:])
                last = 127 if dirn == 0 else 0
                for h in range(4):
                    hp = h // 2; r0 = (h % 2) * 64
                    mm(pss[:, h * 128:(h + 1) * 128], kI[r0:r0 + 64, hp * 128:(hp + 1) * 128],
                       qc[r0:r0 + 64, hp * 128:(hp + 1) * 128])
                tt('dve', sTm[:].rearrange("p (h c) -> p h c", h=4), pss[:].rearrange("p (h c) -> p h c", h=4),
                   msk[:].unsqueeze(1).broadcast_to([128, 4, 128]), ALU.mult)
                if want_out:
                    for h in range(4):
                        hp = h // 2; r0 = (h % 2) * 64
                        mm(po[:, h * 128:(h + 1) * 128], sTm[:, h * 128:(h + 1) * 128], vb[:, h * 128:(h + 1) * 128], True, False)
                        mm(po[:, h * 128:(h + 1) * 128], qc[r0:r0 + 64, hp * 128:(hp + 1) * 128], Sbf[r0:r0 + 64, hp, :], False, True)
                for h in range(4):
                    hp = h // 2; r0 = (h % 2) * 64
                    mm(pD[r0:r0 + 64, hp * 128:(hp + 1) * 128], kS[:, h * 64:(h + 1) * 64], vb[:, h * 128:(h + 1) * 128])
                for hp in range(2):
                    stt(S32[:, hp, :], S32[:, hp, :], Ep[:, hp * 128 + last:hp * 128 + last + 1],
                        pD[:, hp * 128:(hp + 1) * 128], ALU.mult, ALU.add)
                cp('act', Sbf[:], S32[:])
                if want_out and first_pass:
                    cp('act', OF[:, n, :], po[:])
                if want_out and not first_pass:
                    tt('dve', ot[:], po[:], OF[:, n, :], ALU.add)
                    tt('dve', osq[:], ot[:], ot[:], ALU.mult)
                    reduce_sum(ssh[:], osq[:].rearrange("p (h c) -> p h c", h=4))
                    rsqrt_col(rsh[:], ssh[:], 1.0 / 128, tmp4[:])
                    tt('dve', on[:].rearrange("p (h c) -> p h c", h=4), ot[:].rearrange("p (h c) -> p h c", h=4),
                       rsh[:].unsqueeze(2).broadcast_to([128, 4, 128]), ALU.mult)
                    tt('dve', on[:].rearrange("p (h c) -> p h c", h=4), on[:].rearrange("p (h c) -> p h c", h=4),
                       gng[:].unsqueeze(1).broadcast_to([128, 4, 128]), ALU.mult)
                    act(sg[:], pgu[:], AF.Silu)
                    tt('dve', go[:], on[:], sg[:], ALU.mult)
                    for k4 in range(4):
                        tr(pT[:, k4 * 128:(k4 + 1) * 128], go[:, k4 * 128:(k4 + 1) * 128], ident_b[:])
                    cp('act', mixg[:, :, n * 128:(n + 1) * 128], pT[:, 0:512].rearrange("p (k c) -> p k c", k=4))

            for dirn in range(2):
                memset('dve', S32[:], 0.0)
                memset('dve', Sbf[:], 0.0)
                order_c = list(range(NCT)) if dirn == 0 else list(reversed(range(NCT)))
                order_x = list(range(NT)) if dirn == 0 else list(reversed(range(NT)))
                for n in order_c:
                    gla_tile(ctx, UC_S, n, dirn, False, dirn == 0)
                for n in order_x:
                    gla_tile(x, U_S, n, dirn, True, dirn == 0)
            if b == 0:
                dump("mixg", mixg[:], [128, 4, T], BF16)
        P.barrier()
        if upto < 3:
            seqst.close()
            continue
        mixs = sb(seqst, f"mixs{b}", [128, 4, NSET, 128], BF16)
        with ExitStack() as st:
            TOw = sb(st, "TOw", [128, 32, 128], BF16); WSw = sb(st, "WSw", [128, 32, 2, 2, 64], BF16)
            WOw = sb(st, "WOw", [128, 32, 2, 128], BF16); A8w = sb(st, "A8w", [128, 2, 2, 32])
            GLw = sb(st, "GLw", [128, 4, 512], BF16); glb = sb(st, "glb", [128, 4])
            UF = [sb(st, f"UF{g}", [128, NK], BF16) for g in range(32)]
            XH = sb(st, "XH", [128, NK, 2, 32], BF16)
            Xs = [sb(st, f"Xs{i}", [128, 2, 32]) for i in range(2)]
            s1 = sb(st, "s1", [128, 2, 32]); s2 = sb(st, "s2", [128, 2, 32])
            stu = ExitStack()
            Ucm = sb(stu, "Ucm", [128, NCTL, 32, 128], BF16); Ucc = sb(stu, "Ucc", [NKC, 32, 128], BF16); Ustg = sb(stu, "Ustg", [128, 4096], BF16)
            pUF = ps(st, "pUF", [128, 1024], BF16); pSI = [ps(st, f"pSI{i}", [128, 512]) for i in range(2)]
            py = ps(st, "py", [128, 512]); pT2 = ps(st, "pT2", [128, 1024], BF16)
            pgl = [ps(st, f"pgl{i}", [128, 512]) for i in range(2)]
            dma(TOw[:].rearrange("p g c -> p (g c)"), TOEP_S)
            dma(WSw[:].rearrange("p g d r c -> p (g d r c)"), WS_S)
            dma(WOw[:].rearrange("p g r c -> p (g r c)"), WO_S)
            dma(A8w[:].rearrange("p a r g -> p (a r g)"), A8_S)
            dma(GLw[:], GLU_BF.rearrange("(kc p) n -> p kc n", p=128))
            dma(glb[:], s5_glu_b.rearrange("o (kc p) -> p (o kc)", p=128), **NCK)
            for ct in range(NCTL):
                dma(Ustg[0:CPT], U_S[b, ct * 8 * CPT:(ct + 1) * 8 * CPT, :].rearrange("(c j) ch -> c (j ch)", j=8))
                cp('dve', Ucm[0:CPT, ct].rearrange("c g (j h) -> c g j h", j=8),
                   Ustg[0:CPT].rearrange("c (j g h) -> c g j h", j=8, g=32))
            dma(Ustg[0:NKC], UC_S[b].rearrange("(c j) ch -> c (j ch)", j=8))
            cp('dve', Ucc[:].rearrange("c g (j h) -> c g j h", j=8), Ustg[0:NKC].rearrange("c (j g h) -> c g j h", j=8, g=32))
            for g in range(32):
                tr(pUF[:, 0:NKC], Ucc[:, g, :], ident_b[0:NKC, 0:NKC])
                for ct in range(NCTL):
                    tr(pUF[:, NKC + ct * CPT:NKC + (ct + 1) * CPT], Ucm[0:CPT, ct, g, :], ident_b[0:CPT, 0:CPT])
                cp('dve', UF[g][:], pUF[:, 0:NK])
                for ri in range(2):
                    for d_ in range(2):
                        mm(pSI[ri][d_ * 64:(d_ + 1) * 64, 0:NK], WSw[:, g, d_, ri, :], UF[g][:])
                    eng = 'act' if ri == 0 else 'dve'
                    cp(eng, XH[0:64, :, ri, g], pSI[ri][0:64, 0:NK])
                    cp(eng, XH[64:128, 0:NKC, ri, g], pSI[ri][64:128, 0:NKC][:, ::-1])
                    cp(eng, XH[64:128, NKC:NK, ri, g], pSI[ri][64:128, NKC:NK][:, ::-1])
            P.barrier()
            stu.close()
            XHN = sb(st, "XHN", [128, NK, 2, 32], BF16)
            zc = sb(st, "zc", [128, 4, 8, 8, 16], BF16)
            gx2 = sb(st, "gx2", [128, 512]); gin = sb(st, "gin", [128, 512]); gsg = sb(st, "gsg", [128, 512])
            sgl = [sb(st, f"sgl{i}", [128, 512], BF16) for i in range(4)]
            memset('dve', Xs[0][:], 0.0)
            AR2 = A8w[:, 0]; AI2 = A8w[:, 1]
            for k in range(NK):
                Xp = Xs[k % 2]; Xn = Xs[(k + 1) % 2]
                tt('dve', s1[:], AR2, Xp[:], ALU.mult)
                tt('dve', s2[:], AI2, Xp[:, ::-1, :], ALU.mult)
                tt('dve', s1[:], s1[:], s2[:], ALU.add)
                tt('dve', Xn[:], s1[:], (XH[:, k], k), ALU.add)
                cp('act', (XH[:, k], k), Xn[:])
            P.barrier()
            nq = 4
            for qi_ in range(nq):
                a0 = qi_ * NK // nq; a1 = (qi_ + 1) * NK // nq
                cp(['dve', 'pool', 'act', 'dve'][qi_], XHN[64:128, a0:a1], XH[64:128, NK - 1 - a0:(NK - 1 - a1 if NK - 1 - a1 >= 0 else None):-1])
            for qi_ in range(nq):
                a0 = qi_ * NKX // nq; a1 = (qi_ + 1) * NKX // nq
                cp(['act', 'dve', 'pool', 'act'][qi_], XHN[0:64, 1 + a0:1 + a1], XH[0:64, NKC - 1 + a0:NKC - 1 + a1])
            for ct in range(NCTL):
                kf0 = NKC - 1 + ct * CPT
                kb0 = NK - 2 - ct * CPT
                for gb in range(8):
                    for gi in range(4):
                        g = gb * 4 + gi
                        o_ = py[0:CPT, gi * 128:(gi + 1) * 128]
                        mm(o_, UF[g][:, NKC + ct * CPT:NKC + (ct + 1) * CPT], TOw[:, g, :], True, False)
                        for ri in range(2):
                            mm(o_, XHN[:, ct * CPT + 1:ct * CPT + 1 + CPT, ri, g], WOw[:, g, ri, :], False, ri == 1)
                    yv = py[0:CPT, :]
                    act(gx2[0:CPT], yv, AF.Square)
                    ts('dve', gx2[0:CPT], gx2[0:CPT], 0.044715, 1.0, ALU.mult, ALU.add)
                    tt('dve', gin[0:CPT], gx2[0:CPT], yv, ALU.mult)
                    act(gsg[0:CPT], gin[0:CPT], AF.Sigmoid, scale=1.5957691216057308)
                    tt('dve', zc[0:CPT, gb // 2, :, (gb % 2) * 4:(gb % 2) * 4 + 4, :].rearrange("c j g h -> c g j h"),
                       gsg[0:CPT].rearrange("c (g j h) -> c g j h", g=4, j=8), yv.rearrange("c (g j h) -> c g j h", g=4, j=8), ALU.mult)
                for kt in range(4):
                    for j in range(8):
                        tr(pT2[:, j * 128:j * 128 + CPT], zc[0:CPT, kt, j].rearrange("c g h -> c (g h)"), ident_b[0:CPT, 0:CPT])
                    cp('act', mixs[:, kt, ct * 8:(ct + 1) * 8, 0:CPT], pT2[:].rearrange("p (j c) -> p j c", j=8)[:, :, 0:CPT])
            for s0 in range(0, NSET, 4):
                for oc in range(4):
                    pg_ = pgl[oc % 2]
                    for kt in range(4):
                        mm(pg_[:], GLw[:, kt, oc * 128:(oc + 1) * 128], mixs[:, kt, s0:s0 + 4, :].rearrange("p s c -> p (s c)"), kt == 0, kt == 3)
                    act(sgl[oc][:], pg_[:], AF.Sigmoid, bias=glb[:, oc:oc + 1])
                for oc in range(4):
                    mv = mixs[:, oc, s0:s0 + 4, :].rearrange("p s c -> p (s c)")
                    tt('dve', mv, mv, sgl[oc][:], ALU.mult)
            if b == 0:
                dump("mixs", mixs[:], [128, 4, NSET, 128], BF16)
        P.barrier()
        if upto >= 4:
          with ExitStack() as st:
            WOb = sb(st, "WOb", [128, 8, D], BF16); RWb = sb(st, "RWb", [128, 8, NE], BF16)
            SGb = sb(st, "SGb", [128, 8, 256], BF16); SUb = sb(st, "SUb", [128, 8, 256], BF16); SDb = sb(st, "SDb", [128, 2, D], BF16)
            G1 = sb(st, "G1", [128, D]); A2bc = sb(st, "A2bc", [128, D]); B2bc = sb(st, "B2bc", [128, D]); G2 = sb(st, "G2", [128, D])
            rbbc = sb(st, "rbbc", [128, NE])
            xts4 = [sb(st, f"xt4{i}", [128, D]) for i in range(2)]
            t1k = sb(st, "t1k", [128, D]); x1 = sb(st, "x1", [128, D]); x1p = sb(st, "x1p", [128, D])
            junk4 = sb(st, "junk4", [128, D], BF16); hx2 = sb(st, "hx2", [128, D], BF16); hx2T = sb(st, "hx2T", [128, 8, 128], BF16)
            ss4 = sb(st, "ss4", [128, 1]); rstd4 = sb(st, "rstd4", [128, 1]); tmpc4 = sb(st, "tmpc4", [128, 1])
            R_ = {n: sb(st, "r_" + n, [128, NE]) for n in ["scores", "biased", "masked", "sel", "selw", "Wd", "pos", "val", "vm", "hi", "pm", "jk"]}
            selb = sb(st, "selb", [128, NE], BF16)
            m8 = sb(st, "m8", [128, 8, 8]); gs = sb(st, "gs", [128, 8]); gm = sb(st, "gm", [128, 8]); gmask = sb(st, "gmask", [128, 8])
            pen = sb(st, "pen", [128, 8]); t8 = sb(st, "t8", [128, 8]); v8 = sb(st, "v8", [128, 8]); w8 = sb(st, "w8", [128, 8])
            den = sb(st, "den", [128, 1]); rden = sb(st, "rden", [128, 1])
            sgate = sb(st, "sgate", [128, 256]); hidT = sb(st, "hidT", [128, 256], BF16)
            p1 = [ps(st, f"p1{i}", [128, 512]) for i in range(2)]
            pT4 = ps(st, "pT4", [128, 1024], BF16); pl = ps(st, "pl", [128, 512]); ptot = ps(st, "ptot", [128, 512])
            pgu2 = ps(st, "pgu2", [128, 512]); psh = [ps(st, f"psh{i}", [128, 512]) for i in range(2)]
            dma(WOb[:], WOUT_BF.rearrange("(kc p) n -> p kc n", p=128))
            dma(RWb[:], RW_BF.rearrange("(kc p) n -> p kc n", p=128))
            dma(SGb[:], SHG_BF.rearrange("(kc p) n -> p kc n", p=128))
            dma(SUb[:], SHU_BF.rearrange("(kc p) n -> p kc n", p=128))
            dma(SDb[:], SHD_BF.rearrange("(kc p) n -> p kc n", p=128))
            dma(G1[:], MODBC[0, b]); dma(A2bc[:], MODBC[1, b]); dma(B2bc[:], MODBC[2, b]); dma(G2[:], MODBC[3, b])
            dma(rbbc[:], router_b.partition_broadcast(128).rearrange("p o d -> p (o d)"))

            def vmax(o, i):
                return P.op('dve', lambda e, o=o, i=i: e.max(out=o, in_=i), [i], [o])

            for sidx in range(NSET):
                ct = sidx // 8; j = sidx % 8
                gset = b * NSET + sidx
                r0 = ct * 1024 + j
                rsl = slice(r0, r0 + 8 * (CPT - 1) + 1, 8)
                xt = xts4[sidx % 2]
                dma(xt[:], x[b, rsl, :])
                for half in range(2):
                    for kc in range(8):
                        lt = mixg[:, kc, rsl] if kc < 4 else mixs[:, kc - 4, sidx, :]
                        mm(p1[half][:], lt, WOb[:, kc, half * 512:(half + 1) * 512], kc == 0, kc == 7)
                for half in range(2):
                    hs_ = slice(half * 512, (half + 1) * 512)
                    tt('dve', t1k[:, hs_], p1[half][:], G1[:, hs_], ALU.mult)
                tt('dve', x1[:], t1k[:], xt[:], ALU.add)
                act(junk4[:], x1[:], AF.Square, accum=ss4[:])
                rsqrt_col(rstd4[:], ss4[:], 1.0 / D, tmpc4[:])
                stt(t1k[:], x1[:], rstd4[:, 0:1], A2bc[:], ALU.mult, ALU.mult)
                tt('dve', hx2[:], t1k[:], B2bc[:], ALU.add)
                dma(HX2_S[b * T + r0:b * T + r0 + 8 * (CPT - 1) + 1:8, :], hx2[:])
                for kc in range(8):
                    tr(pT4[:, kc * 128:(kc + 1) * 128], hx2[:, kc * 128:(kc + 1) * 128], ident_b[:])
                cp('act', hx2T[:].rearrange("p k c -> p (k c)"), pT4[:])
                for kc in range(8):
                    mm(pl[:, 0:NE], hx2T[:, kc, :], RWb[:, kc, :], kc == 0, kc == 7)
                act(R_["scores"][:], pl[:, 0:NE], AF.Sigmoid)
                tt('dve', R_["biased"][:], R_["scores"][:], rbbc[:], ALU.add)
                for g8 in range(8):
                    vmax(m8[:, g8, :], R_["biased"][:, g8 * 32:(g8 + 1) * 32])
                tt('dve', gs[:], m8[:, :, 0], m8[:, :, 1], ALU.add)
                vmax(gm[:], gs[:])
                ts('dve', gmask[:], gs[:], gm[:, 3:4], None, ALU.is_ge)
                ts('dve', pen[:], gmask[:], -1.0, 1e9, ALU.add, ALU.mult)
                tt('dve', R_["masked"][:].rearrange("p (g e) -> p g e", g=8), R_["biased"][:].rearrange("p (g e) -> p g e", g=8),
                   pen[:].unsqueeze(2).broadcast_to([128, 8, 32]), ALU.add)
                vmax(t8[:], R_["masked"][:])
                ts('dve', R_["sel"][:], R_["masked"][:], t8[:, 7:8], None, ALU.is_ge)
                tt('dve', R_["selw"][:], R_["sel"][:], R_["scores"][:], ALU.mult)
                reduce_sum(den[:], R_["selw"][:])
                recip(rden[:], den[:])
                ts('dve', R_["Wd"][:], R_["selw"][:], rden[:, 0:1], 2.5, ALU.mult, ALU.mult)
                cp('dve', selb[:], R_["sel"][:])
                mm(pl[:, NE:2 * NE], SU_b[:], selb[:])
                mm(ptot[:, 0:NE], ones_b[:], selb[:])
                tt('dve', R_["pos"][:], pl[:, NE:2 * NE], cum[:], ALU.add)
                tt('dve', cum[:], cum[:], ptot[:, 0:NE], ALU.add)
                stt(R_["val"][:], R_["pos"][:], float(NE), ecol[:], ALU.mult, ALU.add)
                tt('dve', R_["val"][:], R_["val"][:], R_["sel"][:], ALU.mult)
                vmax(v8[:], R_["val"][:])
                for k in range(8):
                    stt(R_["jk"][:], R_["val"][:], v8[:, k:k + 1], R_["Wd"][:], ALU.is_equal, ALU.mult, accum=w8[:, k:k + 1])
                cp('dve', V8A[:, gset * 8:(gset + 1) * 8], v8[:])
                cp('dve', W8A[:, gset * 8:(gset + 1) * 8], w8[:])
                for oc in range(2):
                    for kc in range(8):
                        mm(pgu2[:, oc * 128:(oc + 1) * 128], SGb[:, kc, oc * 128:(oc + 1) * 128], hx2T[:, kc, :], kc == 0, kc == 7)
                for oc in range(2):
                    for kc in range(8):
                        mm(pgu2[:, 256 + oc * 128:256 + (oc + 1) * 128], SUb[:, kc, oc * 128:(oc + 1) * 128], hx2T[:, kc, :], kc == 0, kc == 7)
                act(sgate[:], pgu2[:, 0:256], AF.Silu)
                tt('dve', hidT[:], sgate[:], pgu2[:, 256:512], ALU.mult)
                for half in range(2):
                    for oc in range(2):
                        mm(psh[half][:], hidT[:, oc * 128:(oc + 1) * 128], SDb[:, oc, half * 512:(half + 1) * 512], oc == 0, oc == 1)
                for half in range(2):
                    hs_ = slice(half * 512, (half + 1) * 512)
                    tt('dve', t1k[:, hs_], psh[half][:], G2[:, hs_], ALU.mult)
                tt('dve', x1p[:], t1k[:], x1[:], ALU.add)
                dma(X1_S[b, rsl, :], x1p[:])
                if b == 0 and sidx == 6:
                    dump("pos6", R_["pos"][:], [128, NE]); dump("sel6", R_["sel"][:], [128, NE]); dump("cum6", cum[:], [128, NE]); dump("vm6", R_["vm"][:], [128, NE]); dump("val6", R_["val"][:], [128, NE])
                if b == 0 and sidx == 0:
                    dump("x1", x1[:], [128, D]); dump("Wd", R_["Wd"][:], [128, NE]); dump("val", R_["val"][:], [128, NE])
                    dump("x1p", x1p[:], [128, D]); dump("w8", w8[:], [128, 8]); dump("v8", v8[:], [128, 8]); dump("sel", R_["sel"][:], [128, NE]); dump("scores", R_["scores"][:], [128, NE]); dump("pos", R_["pos"][:], [128, NE])
        P.barrier()
        seqst.close()
        P.barrier()


    NSA = 2 * NSET * 8
    if upto >= 5:
        with ExitStack() as st:
            c128 = sb(st, "c128", [128, NE]); ovb = sb(st, "ovb", [128, NE]); cs = sb(st, "cs", [128, NE]); obs = sb(st, "obs", [128, NE])
            tiB = sb(st, "tiB", [128, NE], I32); jkB = sb(st, "jkB", [128, NE]); jcol = sb(st, "jcol", [128, 2]); jcoli = sb(st, "jcoli", [128, 2], I32)
            ebf = sb(st, "ebf", [128, 2])
            vf = sb(st, "vf", [128, NSA]); vi = sb(st, "vi", [128, NSA], I32); posi = sb(st, "posi", [128, NSA], I32); ei = sb(st, "ei", [128, NSA], I32)
            ef = sb(st, "ef", [128, NSA]); posf = sb(st, "posf", [128, NSA]); obs8 = sb(st, "obs8", [128, NSA])
            isov = sb(st, "isov", [128, NSA]); qf = sb(st, "qf", [128, NSA]); qi = sb(st, "qi", [128, NSA], I32); q2 = sb(st, "q2i", [128, NSA], I32)
            pov = sb(st, "pov", [128, NSA]); bov = sb(st, "bov", [128, NSA]); Rf = sb(st, "Rf", [128, NSA])

            def tss(o, i, sc, op):
                return P.op('dve', lambda e, o=o, i=i, sc=sc, op=op: e.tensor_single_scalar(out=o, in_=i, scalar=sc, op=op), [i], [o])

            ts('dve', c128[:], cum[:], -128.0, 0.0, ALU.add, ALU.max)
            ts('dve', c128[:], c128[:], 127.0, None, ALU.add)
            cp('dve', tiB[:], c128[:])
            tss(tiB[:], tiB[:], 7, ALU.arith_shift_right)
            cp('dve', ovb[:], tiB[:])
            P.op('dve', lambda e: e.tensor_tensor_scan(out=cs[:], data0=ones_f[:, 0:1].broadcast_to([128, NE]), data1=ovb[:], initial=0.0, op0=ALU.mult, op1=ALU.add), [ovb[:], ones_f[:]], [cs[:]])
            tt('dve', obs[:], cs[:], ovb[:], ALU.subtract)
            iota(jcoli[:], [[128, 2]], 0, 1)
            cp('dve', jcol[:], jcoli[:])
            for h in range(2):
                ts('dve', jkB[:], cs[:], jcol[:, h:h + 1], None, ALU.is_le)
                reduce_sum(ebf[:, h:h + 1], jkB[:])
            cp('dve', EBI[:], ebf[:])
            with ExitStack() as st3:
                dg = sb(st3, "dg", [128, 128]); ebrow = sb(st3, "ebrow", [128, NE]); pcol = sb(st3, "pcol", [128, 1]); pcoli = sb(st3, "pcoli", [128, 1], I32)
                peb = ps(st3, "peb", [128, 512])
                for h in range(2):
                    ts('dve', dg[:], ident_f[:], ebf[:, h:h + 1], None, ALU.mult)
                    mm(peb[:, h * 128:(h + 1) * 128], ones_f[:], dg[:])
                iota(pcoli[:], [[0, 1]], 0, 1)
                cp('dve', pcol[:], pcoli[:])
                ts('dve', ebrow[:], peb[:, 0:NE], 128.0, pcol[:, 0:1], ALU.mult, ALU.add)
                cp('dve', EBW[:], ebrow[:])
                dump("EBW", EBW[:], [128, NE], U32)
            ts('dve', vf[:], V8A[:], -1.0, None, ALU.add)
            cp('dve', vi[:], vf[:])
            tss(posi[:], vi[:], 8, ALU.arith_shift_right)
            tss(ei[:], vi[:], 255, ALU.bitwise_and)
            cp('dve', ef[:], ei[:]); cp('dve', posf[:], posi[:])
            for a in range(NSA):
                stt(jkB[:], ecol0[:], ef[:, a:a + 1], obs[:], ALU.is_equal, ALU.mult, accum=obs8[:, a:a + 1])
            ts('dve', isov[:], posf[:], 128.0, None, ALU.is_ge)
            ts('dve', qf[:], posf[:], -128.0, 0.0, ALU.add, ALU.max)
            cp('dve', qi[:], qf[:])
            tss(q2[:], qi[:], 127, ALU.bitwise_and)
            cp('dve', pov[:], q2[:])
            tss(q2[:], qi[:], 7, ALU.arith_shift_right)
            cp('dve', bov[:], q2[:])
            tt('dve', bov[:], bov[:], obs8[:], ALU.add)
            ts('dve', bov[:], bov[:], float(NE), None, ALU.add)
            tt('dve', pov[:], pov[:], posf[:], ALU.subtract); tt('dve', pov[:], pov[:], isov[:], ALU.mult); tt('dve', pov[:], pov[:], posf[:], ALU.add)
            tt('dve', bov[:], bov[:], ef[:], ALU.subtract); tt('dve', bov[:], bov[:], isov[:], ALU.mult); tt('dve', bov[:], bov[:], ef[:], ALU.add)
            stt(Rf[:], pov[:], float(NBLK), bov[:], ALU.mult, ALU.add)
            ts('dve', Rf[:], Rf[:], 1.0, None, ALU.add)
            cp('dve', IDX[:], Rf[:])
            iota(RIA[:, :, 0], [[T, 2], [1024, NCTL], [1, 8], [0, 8]], 0, 8)
            cp('dve', RIA[:].bitcast(F32)[:, :, 1], W8A[:])
            dump("IDX", IDX[:], [128, NSA], U32); dump("EBI", EBI[:], [128, 2], I32); dump("cum", cum[:], [128, NE])
            for a in range(NSA):
                P.op('pool', lambda e, a=a: e.indirect_dma_start(
                    out=RINFO, out_offset=bass.IndirectOffsetOnAxis(ap=IDX[:, a:a + 1], axis=0),
                    in_=RIA[:, a, :], in_offset=None), [RIA[:], IDX[:]], [(RINFO, a)], dma=True)
        P.barrier()

    if upto >= 6:
        with ExitStack() as st:
            idxall = sb(st, "idxall", [128, NBLK, 2], I32)
            wgf = [sb(st, f"wgf{i}", [128, 8, 256]) for i in range(2)]; wuf = [sb(st, f"wuf{i}", [128, 8, 256]) for i in range(2)]
            wdf = [sb(st, f"wdf{i}", [128, 2, D]) for i in range(2)]
            wgb = [sb(st, f"wgb{i}", [128, 8, 256], BF16) for i in range(2)]; wub = [sb(st, f"wub{i}", [128, 8, 256], BF16) for i in range(2)]
            wdb = [sb(st, f"wdb{i}", [128, 2, D], BF16) for i in range(2)]
            xg = [sb(st, f"xg{i}", [128, D], BF16) for i in range(2)]; xTe = [sb(st, f"xTe{i}", [128, 8, 128], BF16) for i in range(2)]
            sge = [sb(st, f"sge{i}", [128, 256]) for i in range(2)]; hide = [sb(st, f"hide{i}", [128, 256], BF16) for i in range(2)]
            ybe = [sb(st, f"ybe{i}", [128, D], BF16) for i in range(2)]
            pTe = [ps(st, f"pTe{i}", [128, 1024], BF16) for i in range(2)]; pgue = [ps(st, f"pgue{i}", [128, 512]) for i in range(2)]
            pye = [ps(st, f"pye{i}", [128, 512]) for i in range(4)]
            dma(idxall[:].rearrange("p q t -> p (q t)"), RINFO[1:NROW + 1, :].rearrange("(p q) t -> p (q t)", p=128))
            evc = {}
            NBRUN = NBLK if upto >= 7 or T >= 2048 else NBLK

            def wload(o, src, i):
                ov = o.rearrange("p a n -> p (a n)")
                if i < NE:
                    return dma(ov, src[i * 128:(i + 1) * 128, :])
                j = i - NE
                def f(e, ov=ov, src=src, j=j):
                    if 'bc' not in evc:
                        evc['bc'] = e.to_reg(NE * 128 - 1)
                    return e.indirect_dma_start(
                        out=ov, out_offset=None, in_=src, in_offset=bass.IndirectOffsetOnAxis(ap=EBW[:, j:j + 1], axis=0),
                        bounds_check=evc['bc'], oob_is_err=False)
                return P.op('pool', f, [EBW[:], src], [o], dma=True)

            def issue_loads(i):
                bi = i % 2
                wload(wgf[bi][:], exp_wg, i)
                wload(wuf[bi][:], exp_wu, i)
                wload(wdf[bi][:], exp_wd, i)
                P.op('pool', lambda e, i=i, bi=bi: e.indirect_dma_start(
                    out=xg[bi][:], out_offset=None, in_=HX2_S,
                    in_offset=bass.IndirectOffsetOnAxis(ap=idxall[:, i, 0:1].bitcast(U32), axis=0)), [idxall[:], HX2_S], [xg[bi][:]], dma=True)

            issue_loads(0)
            for i in range(NBRUN):
                bi = i % 2
                if i + 1 < NBRUN:
                    issue_loads(i + 1)
                cp('dve', wgb[bi][:], wgf[bi][:]); cp('act', wub[bi][:], wuf[bi][:])
                cp('dve', wdb[bi][:, 0], wdf[bi][:, 0]); cp('act', wdb[bi][:, 1], wdf[bi][:, 1])
                for kc in range(8):
                    tr(pTe[bi][:, kc * 128:(kc + 1) * 128], xg[bi][:, kc * 128:(kc + 1) * 128], ident_b[:])
                cp('act', xTe[bi][:].rearrange("p k c -> p (k c)"), pTe[bi][:])
                for oc in range(2):
                    for kc in range(8):
                        mm(pgue[bi][:, oc * 128:(oc + 1) * 128], wgb[bi][:, kc, oc * 128:(oc + 1) * 128], xTe[bi][:, kc, :], kc == 0, kc == 7)
                for oc in range(2):
                    for kc in range(8):
                        mm(pgue[bi][:, 256 + oc * 128:256 + (oc + 1) * 128], wub[bi][:, kc, oc * 128:(oc + 1) * 128], xTe[bi][:, kc, :], kc == 0, kc == 7)
                act(sge[bi][:], pgue[bi][:, 0:256], AF.Silu)
                tt('dve', hide[bi][:], sge[bi][:], pgue[bi][:, 256:512], ALU.mult)
                wcol = idxall[:, i, 1:2].bitcast(F32)
                for half in range(2):
                    py_ = pye[bi * 2 + half]
                    for oc in range(2):
                        mm(py_[:], hide[bi][:, oc * 128:(oc + 1) * 128], wdb[bi][:, oc, half * 512:(half + 1) * 512], oc == 0, oc == 1)
                    if half == 0:
                        ts('dve', ybe[bi][:, 0:512], py_[:], wcol, None, ALU.mult)
                    else:
                        act(ybe[bi][:, 512:1024], py_[:], AF.Copy, scale=wcol)
                dma(Y_S[1 + i:1 + i + NBLK * 127 + 1:NBLK, :], ybe[bi][:])
        P.barrier()

    if upto >= 6:
        with ExitStack() as st:
            G2b = [sb(st, f"G2b{i}", [128, D]) for i in range(2)]; fgb = sb(st, "fgb", [128, D])
            x1l = [sb(st, f"x1l{i}", [128, D]) for i in range(2)]
            gthA = [[sb(st, f"gth{q}_{i}", [128, D], BF16) for i in range(8)] for q in range(2)]
            accA = [sb(st, f"acc{q}", [128, D]) for q in range(2)]; acc2A = [sb(st, f"accb{q}", [128, D]) for q in range(2)]
            outtA = [sb(st, f"outt{q}", [128, D]) for q in range(2)]; junk6 = sb(st, "junk6", [128, D], BF16)
            ss6 = sb(st, "ss6", [128, 1]); rstd6 = sb(st, "rstd6", [128, 1]); tmpc6 = sb(st, "tmpc6", [128, 1])
            for b in range(2):
                dma(G2b[b][:], MODBC[3, b])
            dma(fgb[:], final_g.partition_broadcast(128).rearrange("p o d -> p (o d)"))
            for b in range(2):
                for sidx in range(NSET):
                    ct = sidx // 8; j = sidx % 8; gset = b * NSET + sidx
                    r0 = ct * 1024 + j
                    rsl = slice(r0, r0 + 8 * (CPT - 1) + 1, 8)
                    xl = x1l[sidx % 2]; gth = gthA[sidx % 2]; acc = accA[sidx % 2]; acc2 = acc2A[sidx % 2]; outt = outtA[sidx % 2]
                    dma(xl[:], X1_S[b, rsl, :])
                    for k in range(8):
                        P.op('pool', lambda e, k=k, gset=gset, g_=gth[k]: e.indirect_dma_start(
                            out=g_[:], out_offset=None, in_=Y_S,
                            in_offset=bass.IndirectOffsetOnAxis(ap=IDX[:, gset * 8 + k:gset * 8 + k + 1], axis=0)),
                            [IDX[:], Y_S], [gth[k][:]], dma=True)
                    tt('dve', acc[:], gth[0][:], gth[1][:], ALU.add)
                    tt('pool', acc2[:], gth[2][:], gth[3][:], ALU.add)
                    tt('dve', acc[:], acc[:], gth[4][:], ALU.add)
                    tt('pool', acc2[:], acc2[:], gth[5][:], ALU.add)
                    tt('dve', acc[:], acc[:], gth[6][:], ALU.add)
                    tt('pool', acc2[:], acc2[:], gth[7][:], ALU.add)
                    tt('dve', acc[:], acc[:], acc2[:], ALU.add)
                    tt('dve', acc[:], acc[:], G2b[b][:], ALU.mult)
                    tt('dve', acc[:], acc[:], xl[:], ALU.add)
                    act(junk6[:], acc[:], AF.Square, accum=ss6[:])
                    rsqrt_col(rstd6[:], ss6[:], 1.0 / D, tmpc6[:])
                    stt(outt[:], acc[:], rstd6[:, 0:1], fgb[:], ALU.mult, ALU.mult)
                    dma(out[b, rsl, :], outt[:])
        P.barrier()

    P.final_wait_all()
    sems = ExitStack()
    esems = {e: sems.enter_context(nc.semaphore("sem_" + e)) for e in Prog.ENG}
    dsems = {'dma': [sems.enter_context(nc.semaphore(f"dsem{i}")) for i in range(NDSEM)],
             'sw': [sems.enter_context(nc.semaphore(f"ssem{i}")) for i in range(NDSEM)]}
    run = P.emit(esems, dsems)
    with nc.Block() as block:
        @block.tensor
        def _(e):
            run('pe')

        @block.vector
        def _(e):
            run('dve')

        @block.scalar
        def _(e):
            run('act')

        @block.gpsimd
        def _(e):
            run('pool')

        @block.sync
        def _(e):
            run('sp')
    sems.close()
    es.close()
    return nc, dbg_outs


INPUT_NAMES = ['x', 'c', 'ctx', 'c_ctx', 'ada_w', 'ada_b', 'norm1_g', 'norm2_g', 'w_in', 'gla_wa_f', 'gla_ba_f',
               'gla_wa_b', 'gla_ba_b', 'gla_norm_g', 's5_lam_re_f', 's5_lam_im_f', 's5_log_step_f', 's5_lam_re_b',
               's5_lam_im_b', 's5_log_step_b', 's5_b_re', 's5_b_im', 's5_c_re_f', 's5_c_im_f', 's5_c_re_b',
               's5_c_im_b', 's5_d', 's5_glu_w', 's5_glu_b', 'w_out', 'router_w', 'router_b', 'exp_w_gate',
               'exp_w_up', 'exp_w_down', 'sh_w_gate', 'sh_w_up', 'sh_w_down', 'final_norm_g']


def make_in_maps(inputs, ncores):
    f = lambda a: np.ascontiguousarray(np.asarray(a, dtype=np.float32))
    shared = {}
    for k in INPUT_NAMES:
        if k in ('x', 'c', 'ctx'):
            continue
        a = f(inputs[k])
        if k == 'c_ctx':
            a = a.reshape(1, D)
        elif k == 'final_norm_g':
            a = a.reshape(1, D)
        else:
            a = a[0]
            if a.ndim == 1:
                a = a.reshape(1, -1)
            if k.startswith('s5_c_'):
                a = a.reshape(512, 64)
            if k in ('exp_w_gate', 'exp_w_up'):
                a = a.reshape(NE, 8, 128, 256).transpose(0, 2, 1, 3).reshape(NE * 128, 2048)
            if k == 'exp_w_down':
                a = a.reshape(NE, 2, 128, 1024).transpose(0, 2, 1, 3).reshape(NE * 128, 2048)
        shared[k] = np.ascontiguousarray(a)
    xs = f(inputs['x']); cs = f(inputs['c']); cx = f(inputs['ctx'])
    maps = []
    for i in range(ncores):
        m = dict(shared)
        m['x'] = np.ascontiguousarray(xs[2 * i:2 * i + 2])
        m['c'] = np.ascontiguousarray(cs[2 * i:2 * i + 2])
        m['ctx'] = np.ascontiguousarray(cx[2 * i:2 * i + 2])
        maps.append(m)
    return maps


def kernel(**inputs):
    ncores = 8
    T = inputs['x'].shape[1]
    CT = inputs['ctx'].shape[1]
    nc, _ = build_program(T=T, CT=CT, NB=2)
    maps = make_in_maps(inputs, ncores)
    res = run_bass_kernel_spmd(nc, maps, core_ids=list(range(ncores)))
    outs = [np.asarray(r["out"], dtype=np.float32) for r in res.results]
    return np.concatenate(outs, axis=0)
```
